# Optimizing a Trainium2 kernel written in Bass

```python
import jax, jax.numpy as jnp
from jax import lax
import numpy as np

D_MODEL = 2048
BATCH = 1
SEQ = 8192
DEPTH = 4

N_MIXERS = 2
N_POOL_LAYERS = (DEPTH + 1) // 2
N_ATTN_LAYERS = DEPTH // 2
EPS = 1e-6

POOL_WINDOWS = (2, 4, 8, 16)
N_POOL_GROUPS = 4
POOL_GROUP_DIM = D_MODEL // N_POOL_GROUPS
POOL_IN = 2 * D_MODEL

N_HEADS = 16
HEAD_DIM = D_MODEL // N_HEADS
ATTN_WIDTH = N_HEADS * HEAD_DIM
ROT_DIM = HEAD_DIM // 4
IDX_HEADS = 16
IDX_DIM = 64
IDX_ROT = IDX_DIM // 4
ROPE_THETA = 500000.0
TOP_K_MAX = 256
Q_BLOCK = 128
ATTN_SPLITS = (ATTN_WIDTH, 2 * ATTN_WIDTH, 3 * ATTN_WIDTH, 4 * ATTN_WIDTH,
               4 * ATTN_WIDTH + IDX_HEADS * IDX_DIM,
               4 * ATTN_WIDTH + IDX_HEADS * IDX_DIM + IDX_DIM)
ATTN_IN = 4 * ATTN_WIDTH + IDX_HEADS * IDX_DIM + IDX_DIM + IDX_HEADS

kernel_name = "hybrid_pool_dsa_adaln_trunk"


def rms_norm(x, g):
    xf = x.astype(jnp.float32)
    y = xf * lax.rsqrt(jnp.mean(xf * xf, axis=-1, keepdims=True) + EPS)
    return (y * g.astype(jnp.float32)).astype(x.dtype)


def rope_partial(x, positions, rot_dim):
    half = rot_dim // 2
    inv_freq = ROPE_THETA ** (-jnp.arange(half, dtype=jnp.float32) * 2.0 / rot_dim)
    ang = positions.astype(jnp.float32)[..., None] * inv_freq
    cos = jnp.cos(ang)[:, :, None, :]
    sin = jnp.sin(ang)[:, :, None, :]
    xf = x.astype(jnp.float32)
    x1, x2, rest = xf[..., :half], xf[..., half:rot_dim], xf[..., rot_dim:]
    out = jnp.concatenate([x1 * cos - x2 * sin, x2 * cos + x1 * sin, rest], axis=-1)
    return out.astype(x.dtype)


def pool_branch(h, w_in, w_grp, layer_scale, w_out):
    B, L, D = h.shape
    v, gate = jnp.split(h @ w_in, 2, axis=-1)
    vf = v.astype(jnp.float32)
    cs = jnp.concatenate([jnp.zeros((B, 1, D), jnp.float32),
                          jnp.cumsum(vf, axis=1)], axis=1)
    t = jnp.arange(L, dtype=jnp.int32)
    outs = []
    for gi, w in enumerate(POOL_WINDOWS):
        sl = slice(gi * POOL_GROUP_DIM, (gi + 1) * POOL_GROUP_DIM)
        lo = jnp.maximum(t + 1 - w, 0)
        window_sum = cs[:, t + 1, sl] - cs[:, lo, sl]
        count = jnp.minimum(t + 1, w).astype(jnp.float32)[None, :, None]
        pooled = (window_sum / count - vf[:, :, sl]).astype(h.dtype)
        outs.append(jnp.einsum('bsc,cd->bsd', pooled, w_grp[gi]))
    mixed = jnp.concatenate(outs, axis=-1) * layer_scale
    return (mixed * jax.nn.silu(gate)) @ w_out


def dsa_branch(h, positions, w_in, w_out):
    B, L, _ = h.shape
    proj = h @ w_in
    q, k, v, gate, qi, ki, wi = jnp.split(proj, ATTN_SPLITS, axis=-1)
    q = rope_partial(q.reshape(B, L, N_HEADS, HEAD_DIM), positions, ROT_DIM)
    k = rope_partial(k.reshape(B, L, N_HEADS, HEAD_DIM), positions, ROT_DIM)
    v = v.reshape(B, L, N_HEADS, HEAD_DIM)
    qi = rope_partial(qi.reshape(B, L, IDX_HEADS, IDX_DIM), positions, IDX_ROT)
    ki = rope_partial(ki.reshape(B, L, 1, IDX_DIM), positions, IDX_ROT)[:, :, 0]
    ki_f = ki.astype(jnp.float32)
    top_k = min(TOP_K_MAX, L // 4)
    nb = L // Q_BLOCK
    key_idx = jnp.arange(L, dtype=jnp.int32)
    idx_scale = (IDX_HEADS ** -0.5) * (IDX_DIM ** -0.5)
    attn_scale = HEAD_DIM ** -0.5

    def to_blocks(a):
        return jnp.moveaxis(a.reshape(B, nb, Q_BLOCK, *a.shape[2:]), 1, 0)

    def block_fn(args):
        qb, qib, wb, tb = args
        s = jnp.einsum('bqhd,bsd->bqhs', qib.astype(jnp.float32), ki_f)
        score = jnp.einsum('bqhs,bqh->bqs', jax.nn.relu(s),
                           wb.astype(jnp.float32)) * idx_scale
        admissible = key_idx[None, None, :] <= tb[None, :, None]
        score = jnp.where(admissible, score, -jnp.inf)
        _, sel = lax.top_k(score, top_k)
        valid = sel <= tb[None, :, None]
        k_sel = jax.vmap(lambda kk, ii: kk[ii])(k, sel)
        v_sel = jax.vmap(lambda vv, ii: vv[ii])(v, sel)
        logits = jnp.einsum('bqhd,bqkhd->bhqk', qb, k_sel).astype(jnp.float32) * attn_scale
        logits = jnp.where(valid[:, None], logits, -jnp.inf)
        p = jax.nn.softmax(logits, axis=-1).astype(v.dtype)
        return jnp.einsum('bhqk,bqkhd->bqhd', p, v_sel)

    t_blocks = key_idx.reshape(nb, Q_BLOCK)
    outs = lax.map(block_fn, (to_blocks(q), to_blocks(qi), to_blocks(wi), t_blocks))
    o = jnp.moveaxis(outs, 0, 1).reshape(B, L, ATTN_WIDTH)
    return (o * jax.nn.silu(gate)) @ w_out


def setup_inputs(seed: int = 0) -> dict:
    key = jax.random.key(seed)
    ks = jax.random.split(key, 13)
    D = D_MODEL
    x = jax.random.normal(ks[0], (BATCH, SEQ, D), jnp.float32)
    c = jax.random.normal(ks[1], (BATCH, D), jnp.float32)
    positions = jnp.broadcast_to(jnp.arange(SEQ, dtype=jnp.int32)[None, :], (BATCH, SEQ))
    norm_g = 1.0 + 0.02 * jax.random.normal(ks[2], (DEPTH, D), jnp.float32)
    mod_w = 0.5 * D ** -0.5 * jax.random.normal(ks[3], (DEPTH, D, 3 * D), jnp.float32)
    mod_b = 0.02 * jax.random.normal(ks[4], (DEPTH, 3 * D), jnp.float32)
    pool_w_in = D ** -0.5 * jax.random.normal(ks[5], (N_POOL_LAYERS, D, POOL_IN), jnp.float32)
    pool_w_grp = POOL_GROUP_DIM ** -0.5 * jax.random.normal(
        ks[6], (N_POOL_LAYERS, N_POOL_GROUPS, POOL_GROUP_DIM, POOL_GROUP_DIM), jnp.float32)
    pool_scale = 1.0 + 0.1 * jax.random.normal(ks[7], (N_POOL_LAYERS, D), jnp.float32)
    pool_w_out = D ** -0.5 * jax.random.normal(ks[8], (N_POOL_LAYERS, D, D), jnp.float32)
    attn_w_in = D ** -0.5 * jax.random.normal(ks[9], (N_ATTN_LAYERS, D, ATTN_IN), jnp.float32)
    attn_w_out = ATTN_WIDTH ** -0.5 * jax.random.normal(
        ks[10], (N_ATTN_LAYERS, ATTN_WIDTH, D), jnp.float32)
    final_g = 1.0 + 0.02 * jax.random.normal(ks[11], (D,), jnp.float32)
    return {"x": x, "c": c, "positions": positions, "norm_g": norm_g,
            "mod_w": mod_w, "mod_b": mod_b, "pool_w_in": pool_w_in,
            "pool_w_grp": pool_w_grp, "pool_scale": pool_scale,
            "pool_w_out": pool_w_out, "attn_w_in": attn_w_in,
            "attn_w_out": attn_w_out, "final_g": final_g}


def reference(x, c, positions, norm_g, mod_w, mod_b, pool_w_in, pool_w_grp,
              pool_scale, pool_w_out, attn_w_in, attn_w_out, final_g):
    cond = jax.nn.silu(c)
    for i in range(DEPTH):
        mod = cond @ mod_w[i] + mod_b[i]
        shift, scale, gate = jnp.split(mod, 3, axis=-1)
        h = rms_norm(x, norm_g[i]) * (1.0 + scale[:, None, :]) + shift[:, None, :]
        j = i // N_MIXERS
        if i % N_MIXERS == 0:
            out = pool_branch(h, pool_w_in[j], pool_w_grp[j], pool_scale[j], pool_w_out[j])
        else:
            out = dsa_branch(h, positions, attn_w_in[j], attn_w_out[j])
        x = x + gate[:, None, :] * out
    return rms_norm(x, final_g)
```

```python
import numpy as np
import ml_dtypes
from contextlib import ExitStack
import concourse.bass as bass
import concourse.mybir as mybir
from concourse.bass_utils import run_bass_kernel_spmd

F32 = mybir.dt.float32
BF16 = mybir.dt.bfloat16
I32 = mybir.dt.int32
ALU = mybir.AluOpType
AF = mybir.ActivationFunctionType
AX = mybir.AxisListType


class Buf:
    __slots__ = ("name", "lw", "rd", "dsem", "dval")

    def __init__(self, name):
        self.name = name
        self.lw = None
        self.rd = []
        self.dsem = None
        self.dval = 0


class Eng:
    def __init__(self, name):
        self.name = name
        self.sem = None
        self.count = 0
        self.ops = []
        self.waited = {}


class Ctx:
    def __init__(self, nc, es):
        self.nc = nc
        self.es = es
        self.es0 = es
        self.eng = {n: Eng(n) for n in ("pe", "act", "dve", "pool", "sp")}
        for n in ("pe", "act", "dve", "pool"):
            self.eng[n].sem = es.enter_context(nc.semaphore("s_" + n))
        self.nsem = 4
        self.outbufs = []
        self.dmabufs = []

    def buf(self, name):
        return Buf(name)

    def sbuf(self, name, shape, dt):
        t = self.es.enter_context(self.nc.sbuf_tensor("sb_" + name, list(shape), dt))
        return t, Buf(name)

    def psum(self, name, shape, dt):
        t = self.es.enter_context(self.nc.psum_tensor("pp_" + name, list(shape), dt))
        return t, Buf(name)

    def _wait(self, C, dep):
        if dep[0] == "eng":
            E, idx = dep[1], dep[2]
            assert E.count >= idx, f"dep on unsignaled instr {E.name} {idx} {E.count}"
            sem, val = E.sem, idx
        else:
            sem, val = dep[1], dep[2]
        k = id(sem)
        if C.waited.get(k, 0) >= val:
            return
        C.waited[k] = val
        C.ops.append(("wait", sem, val))

    def _deps(self, C, reads, writes, is_dma):
        deps = []
        for r in reads:
            if r.lw is not None:
                d = r.lw
                if d[0] == "eng" and d[1] is C and C.name == "pe":
                    continue
                deps.append(d)
        for w in writes:
            if w.lw is not None:
                d = w.lw
                if not (d[0] == "eng" and d[1] is C and not is_dma):
                    deps.append(d)
            for d in w.rd:
                if not (d[0] == "eng" and d[1] is C and not is_dma):
                    deps.append(d)
        for d in deps:
            self._wait(C, d)

    def op(self, eng, meth, reads, writes, *args, signal=True, **kw):
        C = self.eng[eng]
        self._deps(C, reads, writes, False)
        if signal:
            C.count += 1
            idx = C.count
        else:
            idx = C.count + 1
        C.ops.append(("inst", meth, args, kw, signal))
        rec = ("eng", C, idx)
        for r in reads:
            r.rd.append(rec)
        for w in writes:
            w.lw = rec
            w.rd = []

    def dma(self, q, out, in_, reads, wbuf, nowaw=False, semof=None, **kw):
        C = self.eng[q]
        sb = semof if semof is not None else wbuf
        if nowaw:
            sv = wbuf.lw
            wbuf.lw = None
            self._deps(C, reads, [wbuf], True)
            wbuf.lw = sv
        else:
            self._deps(C, reads, [wbuf], True)
        if sb.dsem is None:
            sb.dsem = self.es0.enter_context(self.nc.semaphore("d_" + sb.name))
            self.nsem += 1
            self.dmabufs.append(sb)
        sb.dval += 16
        C.ops.append(("dma", out, in_, kw, sb.dsem))
        rec = ("dma", sb.dsem, sb.dval)
        for r in reads:
            r.rd.append(rec)
        wbuf.lw = rec
        wbuf.rd = []

    def barrier(self):
        comp = [self.eng[n] for n in ("pe", "act", "dve", "pool")]
        for C in self.eng.values():
            for E in comp:
                if E is not C and E.count > 0:
                    self._wait(C, ("eng", E, E.count))
            for b in self.dmabufs:
                self._wait(C, ("dma", b.dsem, b.dval))

    def emit(self):
        nc = self.nc
        C = self.eng["sp"]
        for b in self.dmabufs:
            self._wait(C, ("dma", b.dsem, b.dval))
        hwmap = {"pe": "tensor", "act": "scalar", "dve": "vector", "pool": "gpsimd", "sp": "sync"}
        with nc.Block() as block:
            for n, E in self.eng.items():
                def body(hw, E=E):
                    for o in E.ops:
                        if o[0] == "wait":
                            hw.wait_ge(o[1], o[2])
                        elif o[0] == "inst":
                            ins = getattr(hw, o[1])(*o[2], **o[3])
                            if o[4]:
                                ins.then_inc(E.sem, 1)
                        else:
                            hw.dma_start(out=o[1], in_=o[2], **o[3]).then_inc(o[4], 16)
                getattr(block, hwmap[n])(body)


D = 2048
NB = 8
NCORE = 8
HC = 144
NCOL = NB * HC
EPS = 1e-6
WINS = (2, 4, 8, 16)


def make_consts(c, nc):
    K = {}
    idt, idb = c.sbuf("ident", [128, 128], BF16)
    c.op("pool", "memset", [], [idb], idt[:], 0.0)
    c.op("pool", "affine_select", [idb], [idb], out=idt[:], in_=idt[:], pattern=[[-1, 128]],
         compare_op=ALU.not_equal, fill=1.0, base=0, channel_multiplier=1)
    K["ident"] = (idt, idb)
    return K


def mod_step(c, nc, ccol_d, modw_d, modb_d, g_d, ps, outs, gbc_pair, mbb, ws):
    ccol, ccb = c.sbuf("ccol", [128, 16], F32)
    cond, condb = c.sbuf("cond", [128, 16], F32)
    crep, crepb = c.sbuf("crep", [128, 16, 128], BF16)
    gbc, gbcb = gbc_pair
    c.dma("sp", ccol[:], ccol_d, [], ccb)
    c.op("act", "activation", [ccb], [condb], out=cond[:], in_=ccol[:], func=AF.Silu)
    c.op("dve", "tensor_copy", [condb], [crepb], out=crep[:],
         in_=cond[:].unsqueeze(2).to_broadcast([128, 16, 128]))
    c.dma("sp", gbc[:], g_d.partition_broadcast(128), [], gbcb)
    mw = modw_d.rearrange("(kc p) n -> p kc n", p=128)
    names = ["shift", "geff", "gate"]
    for nch in range(12):
        wt, wb = ws[nch % 2]
        bt, bb = mbb[nch % 2]
        wt = wt[:].rearrange("p (k n) -> p k n", k=16)
        c.dma("pool", wt, mw[:, :, nch * 512:(nch + 1) * 512], [], wb)
        c.dma("sp", bt[:], modb_d[:, nch * 512:(nch + 1) * 512].partition_broadcast(128), [], bb)
        pt, pb = ps[nch % 2]
        for kc in range(16):
            c.op("pe", "matmul", [crepb, wb], [pb], pt[:], crep[:, kc, :], wt[:, kc, :],
                 start=(kc == 0), stop=(kc == 15), signal=(kc == 15))
        ot, ob = outs[names[nch // 4]]
        sl = slice((nch % 4) * 512, (nch % 4 + 1) * 512)
        c.op("dve", "tensor_tensor", [pb, bb], [ob], out=ot[:, sl], in0=pt[:], in1=bt[:], op=ALU.add)
    ot, ob = outs["geff"]
    c.op("dve", "scalar_tensor_tensor", [ob, gbcb], [ob], out=ot[:], in0=ot[:], scalar=1.0, in1=gbc[:],
         op0=ALU.add, op1=ALU.mult)


def norm_rows(c, xt, xb, outs, tmp, hb_t, hb_b):
    sq, sqb = tmp["sq"]
    ss, ssb = tmp["ss"]
    rs, rsb = tmp["rs"]
    h1, h1b = tmp["h1"]
    c.op("act", "activation", [xb], [sqb, ssb], out=sq[:], in_=xt, func=AF.Square, accum_out=ss[:])
    c.op("dve", "tensor_scalar", [ssb], [rsb], out=rs[:], in0=ss[:], scalar1=1.0 / D, scalar2=EPS,
         op0=ALU.mult, op1=ALU.add)
    c.op("act", "activation", [rsb], [rsb], out=rs[:], in_=rs[:], func=AF.Sqrt)
    c.op("dve", "reciprocal", [rsb], [rsb], out=rs[:], in_=rs[:])
    gt, gb = outs["geff"]
    st, sb = outs["shift"]
    c.op("dve", "scalar_tensor_tensor", [xb, rsb, gb], [h1b], out=h1[:], in0=xt, scalar=rs[:, 0:1], in1=gt[:],
         op0=ALU.mult, op1=ALU.mult)
    c.op("pool", "tensor_tensor", [h1b, sb], [hb_b], out=hb_t, in0=h1[:], in1=st[:], op=ALU.add)


def build_pool(stop=None):
    nc = bass.Bass("TRN2", target_bir_lowering=False)
    dt = nc.dram_tensor
    x_d = dt("xs", [NB, 128, D], F32, kind="ExternalInput").ap()
    xh_d = dt("xh", [128, D], F32, kind="ExternalInput").ap()
    tok_d = dt("tokrow", [1, NCOL], F32, kind="ExternalInput").ap()
    ccol_d = dt("ccol", [128, 16], F32, kind="ExternalInput").ap()
    modw_d = dt("modw", [D, 3 * D], F32, kind="ExternalInput").ap()
    modb_d = dt("modb", [1, 3 * D], F32, kind="ExternalInput").ap()
    g_d = dt("g", [1, D], F32, kind="ExternalInput").ap()
    win_d = dt("win", [D, 2 * D], F32, kind="ExternalInput").ap()
    wgrp_d = dt("wgrp", [4, 512, 512], F32, kind="ExternalInput").ap()
    ls_d = dt("lscol", [128, 16], F32, kind="ExternalInput").ap()
    wout_d = dt("wout", [D, D], F32, kind="ExternalInput").ap()
    xo_d = dt("xo", [NB, 128, D], F32, kind="ExternalOutput").ap()
    with ExitStack() as es:
        c = Ctx(nc, es)
        K = make_consts(c, nc)
        idt, idb = K["ident"]
        ps = [c.psum(f"ps{i}", [128, 512], F32) for i in range(8)]
        pTs = [(ps[6][0][:].bitcast(BF16), ps[6][1]), (ps[7][0][:].bitcast(BF16), ps[7][1])]
        outs = {n: c.sbuf(n + "_bc", [128, D], F32) for n in ("shift", "geff", "gate")}
        wsl = [c.sbuf(f"wsl{i}", [128, 16 * 512], BF16) for i in range(3)]
        xin = [c.sbuf(f"xin{i}", [128, D], F32) for i in range(2)]
        h1p = c.sbuf("h1", [128, D], F32)
        gbcp = c.sbuf("gbc", [128, D], F32)
        xo = [c.sbuf(f"xo{i}", [128, 512], F32) for i in range(2)]
        xr = [c.sbuf(f"xr{i}", [128, 512], F32) for i in range(2)]
        mod_step(c, nc, ccol_d, modw_d, modb_d, g_d, ps, outs, gbcp, xr, wsl)
        if stop == "mod":
            dbg_d = dt("dbg", [3, 128, D], F32, kind="ExternalOutput").ap()
            dbb = c.buf("dbg"); c.outbufs.append(dbb)
            for i, n in enumerate(("shift", "geff", "gate")):
                c.dma("sp", dbg_d[i], outs[n][0][:], [outs[n][1]], dbb)
            c.emit()
            return nc

        hT, hTb = c.sbuf("hT", [128, 16, NCOL], BF16)
        hbs = [c.sbuf(f"hb{i}", [128, D], BF16) for i in range(2)]
        tmp = {"sq": c.sbuf("sq", [128, D], BF16), "ss": c.sbuf("ss", [128, 1], F32),
               "rs": c.sbuf("rs", [128, 1], F32), "h1": h1p}
        for rb in range(NB + 1):
            xt, xb = xin[rb % 2]
            c.dma("sp", xt[:], x_d[rb] if rb < NB else xh_d, [], xb)
            ht, hb = hbs[rb % 2]
            norm_rows(c, xt[:], xb, outs, tmp, ht[:], hb)
            for q in range(4):
                half = q % 2
                pT, pb = pTs[half]
                for f in range(4):
                    fc = q * 4 + f
                    c.op("pe", "transpose", [hb, idb], [pb], pT[:, f * 128:(f + 1) * 128],
                         ht[:, fc * 128:(fc + 1) * 128], idt[:], signal=(f == 3))
                src = pT[:, 0:512]
                if rb < NB:
                    dst = hT[:, q * 4:(q + 1) * 4, rb * HC + 16: rb * HC + 144]
                    srcv = src.rearrange("p (f t) -> p f t", f=4)
                else:
                    dst = hT[:, q * 4:(q + 1) * 4, :].rearrange("p f (b c) -> p f b c", c=HC)[:, :, :, 0:16]
                    srcv = src.rearrange("p (f b r) -> p f b r", f=4, b=NB)
                eng = "act" if q % 2 == 0 else "dve"
                if eng == "act":
                    c.op("act", "activation", [pb], [hTb], out=dst, in_=srcv, func=AF.Copy)
                else:
                    c.op("dve", "tensor_copy", [pb], [hTb], out=dst, in_=srcv)

        if stop == "N":
            dbg_d = dt("dbg", [128, 16 * NCOL], BF16, kind="ExternalOutput").ap()
            dbb = c.buf("dbg"); c.outbufs.append(dbb)
            c.dma("sp", dbg_d, hT[:].rearrange("p k c -> p (k c)"), [hTb], dbb)
            c.emit()
            return nc
        tokbc, tokb = c.sbuf("tokbc", [128, NCOL], F32)
        c.dma("sp", tokbc[:], tok_d.partition_broadcast(128), [], tokb)
        vmask, vmb = c.sbuf("vmask", [128, NB, 16], F32)
        tok3 = tokbc[:].rearrange("p (b c) -> p b c", c=HC)
        c.op("dve", "tensor_scalar", [tokb], [vmb], out=vmask[:], in0=tok3[:, :, 0:16], scalar1=0.0, scalar2=None,
             op0=ALU.is_ge)
        cnt, cntb = c.sbuf("cnt", [128, NB, 128], F32)
        lscol, lsb = c.sbuf("lscol", [128, 16], F32)
        c.dma("sp", lscol[:], ls_d, [], lsb)
        wgs = [c.sbuf(f"wgs{i}", [128, 4, 512], BF16) for i in range(2)]
        def v3(pair):
            return (pair[0][:, 0:NCOL].rearrange("p (b c) -> p b c", c=HC), pair[1])
        vs_ = [v3(xin[0]), v3(xin[1])]
        lv = [v3(h1p), v3(gbcp)]
        pooled, pooledb = c.sbuf("pooled", [128, 4, NB * 128], BF16)
        sg = [c.sbuf(f"sg{i}", [128, 512], F32) for i in range(2)]
        uT = [(outs[n][0][:].bitcast(BF16).rearrange("p (k t) -> p k t", k=4), outs[n][1]) for n in ("shift", "geff")]
        xreg = {(b, mq): c.buf(f"xreg{b}_{mq}") for b in range(NB) for mq in range(2)}
        c.outbufs += list(xreg.values())
        winv = win_d.rearrange("(kc p) n -> p kc n", p=128)
        woutv = wout_d.rearrange("(cc p) m -> p cc m", p=128)
        wslot_i = 0
        xcount = 0
        for gi in range(4):
            w = WINS[gi]
            c.op("dve", "tensor_scalar", [tokb], [cntb], out=cnt[:], in0=tok3[:, :, 16:144], scalar1=1.0,
                 scalar2=float(w), op0=ALU.add, op1=ALU.min)
            c.op("dve", "reciprocal", [cntb], [cntb], out=cnt[:], in_=cnt[:])
            Wv_t, Wv_b = wsl[wslot_i % 3]; wslot_i += 1
            Wv = Wv_t[:].rearrange("p (k n) -> p k n", k=16)
            c.dma("pool", Wv, winv[:, :, gi * 512:(gi + 1) * 512], [], Wv_b)
            Wg_t, Wg_b = wsl[wslot_i % 3]; wslot_i += 1
            Wg = Wg_t[:].rearrange("p (k n) -> p k n", k=16)
            c.dma("pool", Wg, winv[:, :, D + gi * 512: D + (gi + 1) * 512], [], Wg_b)
            wgt, wgb = wgs[gi % 2]
            c.dma("pool", wgt[:], wgrp_d[gi].rearrange("(cc p) n -> p cc n", p=128), [], wgb)
            for fl in range(4):
                for kc in range(16):
                    for j in range(3):
                        pt, pb = ps[j]
                        c.op("pe", "matmul", [Wv_b, hTb], [pb], pt[:, 0:384], Wv[:, kc, fl * 128:(fl + 1) * 128],
                             hT[:, kc, j * 384:(j + 1) * 384], start=(kc == 0), stop=(kc == 15),
                             signal=(kc == 15))
                vt, vb = vs_[fl % 2]
                vflat = xin[fl % 2][0][:, 0:NCOL]
                for j in range(3):
                    pt, pb = ps[j]
                    c.op("act", "activation", [pb], [vb], out=vflat[:, j * 384:(j + 1) * 384], in_=pt[:, 0:384],
                         func=AF.Copy)
                c.op("dve", "tensor_tensor", [vb, vmb], [vb], out=vt[:, :, 0:16], in0=vt[:, :, 0:16], in1=vmask[:],
                     op=ALU.mult)
                cur, curb = vt, vb
                off = 0
                step = 1
                li = 0
                while step < w:
                    off += step
                    nt, nb_ = lv[li % 2]
                    li += 1
                    c.op("dve", "tensor_tensor", [curb], [nb_], out=nt[:, :, off:HC], in0=cur[:, :, off:HC],
                         in1=cur[:, :, off - step:HC - step], op=ALU.add)
                    cur, curb = nt, nb_
                    step *= 2
                nt, nb_ = lv[li % 2]
                c.op("dve", "tensor_tensor", [curb, cntb], [nb_], out=nt[:, :, 16:HC], in0=cur[:, :, 16:HC],
                     in1=cnt[:], op=ALU.mult)
                c.op("pool", "tensor_tensor", [nb_, vb], [pooledb],
                     out=pooled[:, fl, :].rearrange("p (b t) -> p b t", b=NB), in0=nt[:, :, 16:HC],
                     in1=vt[:, :, 16:HC], op=ALU.subtract)
            ut, ub = uT[gi % 2]
            for nl in range(4):
                nchunk = gi * 4 + nl
                for j in range(2):
                    pm, pmb = ps[3 + j]
                    for cc in range(4):
                        c.op("pe", "matmul", [wgb, pooledb], [pmb], pm[:], wgt[:, cc, nl * 128:(nl + 1) * 128],
                             pooled[:, cc, j * 512:(j + 1) * 512], start=(cc == 0), stop=(cc == 3),
                             signal=(cc == 3))
                    pg, pgb = ps[5 + j]
                    for kc in range(16):
                        rhs = hT[:, kc, :].rearrange("p (b c) -> p b c", c=HC)[:, j * 4:(j + 1) * 4, 16:HC]
                        c.op("pe", "matmul", [Wg_b, hTb], [pgb], pg[:], Wg[:, kc, nl * 128:(nl + 1) * 128], rhs,
                             start=(kc == 0), stop=(kc == 15), signal=(kc == 15))
                    st_, sb_ = sg[j]
                    c.op("act", "activation", [pgb], [sb_], out=st_[:], in_=pg[:], func=AF.Silu)
                    c.op("dve", "scalar_tensor_tensor", [pmb, lsb, sb_], [ub], out=ut[:, nl, j * 512:(j + 1) * 512],
                         in0=pm[:], scalar=lscol[:, nchunk:nchunk + 1], in1=st_[:], op0=ALU.mult, op1=ALU.mult)
            gt_, gb_ = outs["gate"]
            for mh in range(2):
                Wo_t, Wo_b = wsl[wslot_i % 3]; wslot_i += 1
                Wo = Wo_t[:, 0:4096].rearrange("p (k m) -> p k m", k=4)
                c.dma("pool", Wo, woutv[:, gi * 4:(gi + 1) * 4, mh * 1024:(mh + 1) * 1024], [], Wo_b)
                for b in range(NB):
                    for mq in range(2):
                        mcol = mh * 1024 + mq * 512
                        po, pob = ps[(b * 2 + mq) % 3]
                        for cc in range(4):
                            c.op("pe", "matmul", [ub, Wo_b], [pob], po[:], ut[:, cc, b * 128:(b + 1) * 128],
                                 Wo[:, cc, mq * 512:(mq + 1) * 512], start=(cc == 0), stop=(cc == 3),
                                 signal=(cc == 3))
                        xrt, xrb = xr[xcount % 2]
                        xot, xob = xo[xcount % 2]
                        xcount += 1
                        src = x_d if gi == 0 else xo_d
                        c.dma("sp", xrt[:], src[b, :, mcol:mcol + 512], [xreg[(b, mq)]] if gi > 0 else [], xrb)
                        c.op("dve", "tensor_tensor", [pob, gb_], [xob], out=xot[:], in0=po[:],
                             in1=gt_[:, mcol:mcol + 512], op=ALU.mult)
                        c.op("pool", "tensor_tensor", [xob, xrb], [xob], out=xot[:], in0=xot[:], in1=xrt[:],
                             op=ALU.add)
                        c.dma("sp", xo_d[b, :, mcol:mcol + 512], xot[:], [xob], xreg[(b, mq)])
        c.emit()
    return nc


ATT_IN = 9296
NH = 16
TWO_PI = 6.283185307179586


def rope_tables(c, pos_d, invf_d):
    posi, posib = c.sbuf("posi", [128, NB], I32)
    posf, posfb = c.sbuf("posf", [128, NB], F32)
    invf, invfb = c.sbuf("invf", [128, 24], F32)
    cs, csb = c.sbuf("cs", [128, NB, 2, 24], F32)
    yk, ykb = c.sbuf("yk", [128, NB, 2, 24], I32)
    yf, yfb = c.sbuf("yf", [128, NB, 2, 24], F32)
    m1, m1b = c.sbuf("rm1", [128, NB, 2, 24], F32)
    c.dma("sp", posi[:], pos_d, [], posib)
    c.dma("sp", invf[:], invf_d.partition_broadcast(128), [], invfb)
    c.op("dve", "tensor_copy", [posib], [posfb], out=posf[:], in_=posi[:])
    for j in range(NB):
        c.op("dve", "tensor_scalar", [posfb, invfb], [csb], out=cs[:, j, 1, :], in0=invf[:], scalar1=posf[:, j:j + 1],
             scalar2=1.0 / TWO_PI, op0=ALU.mult, op1=ALU.mult)
    c.op("dve", "tensor_scalar", [csb], [csb], out=cs[:, :, 0, :], in0=cs[:, :, 1, :], scalar1=0.25, scalar2=None,
         op0=ALU.add)
    c.op("dve", "tensor_copy", [csb], [ykb], out=yk[:], in_=cs[:])
    c.op("dve", "tensor_copy", [ykb], [yfb], out=yf[:], in_=yk[:])
    c.op("dve", "tensor_tensor", [csb, yfb], [csb], out=cs[:], in0=cs[:], in1=yf[:], op=ALU.subtract)
    c.op("dve", "tensor_scalar", [csb], [m1b], out=m1[:], in0=cs[:], scalar1=0.5, scalar2=None, op0=ALU.is_gt)
    c.op("dve", "tensor_tensor", [csb, m1b], [csb], out=cs[:], in0=cs[:], in1=m1[:], op=ALU.subtract)
    c.op("dve", "tensor_scalar", [csb], [m1b], out=m1[:], in0=cs[:], scalar1=-0.5, scalar2=None, op0=ALU.is_lt)
    c.op("dve", "tensor_tensor", [csb, m1b], [csb], out=cs[:], in0=cs[:], in1=m1[:], op=ALU.add)
    c.op("act", "activation", [csb], [csb], out=cs[:], in_=cs[:], func=AF.Sin, scale=TWO_PI)
    return cs, csb


def build_a1(stop=None):
    nc = bass.Bass("TRN2", target_bir_lowering=False)
    dt = nc.dram_tensor
    x_d = dt("xs", [NB, 128, D], F32, kind="ExternalInput").ap()
    ccol_d = dt("ccol", [128, 16], F32, kind="ExternalInput").ap()
    modw_d = dt("modw", [D, 3 * D], F32, kind="ExternalInput").ap()
    modb_d = dt("modb", [1, 3 * D], F32, kind="ExternalInput").ap()
    g_d = dt("g", [1, D], F32, kind="ExternalInput").ap()
    win_d = dt("win", [D, ATT_IN], F32, kind="ExternalInput").ap()
    pos_d = dt("posc", [128, NB], I32, kind="ExternalInput").ap()
    invf_d = dt("invf", [1, 24], F32, kind="ExternalInput").ap()
    QT_d = dt("QT", [NH, 128, NB * 128], BF16, kind="ExternalOutput").ap()
    KT_d = dt("KT", [NH, 128, NB * 128], BF16, kind="ExternalOutput").ap()
    SG_d = dt("SG", [NH, 128, NB * 128], BF16, kind="ExternalOutput").ap()
    V_d = dt("V", [NB * 128, D], BF16, kind="ExternalOutput").ap()
    QI_d = dt("QIT", [8, 128, NB * 128], BF16, kind="ExternalOutput").ap()
    KI_d = dt("KIT", [64, NB * 128], BF16, kind="ExternalOutput").ap()
    WI_d = dt("WI", [128, NB, 16], F32, kind="ExternalOutput").ap()
    GB_d = dt("GBC", [128, D], F32, kind="ExternalOutput").ap()
    with ExitStack() as es:
        c = Ctx(nc, es)
        K = make_consts(c, nc)
        idt, idb = K["ident"]
        ps = [c.psum(f"ps{i}", [128, 512], F32) for i in range(8)]
        pTs = [(ps[6][0][:].bitcast(BF16), ps[6][1]), (ps[7][0][:].bitcast(BF16), ps[7][1])]
        outs = {n: c.sbuf(n + "_bc", [128, D], F32) for n in ("shift", "geff", "gate")}
        wsl = [c.sbuf(f"wsl{i}", [128, 16 * 512], BF16) for i in range(3)]
        xin = [c.sbuf(f"xin{i}", [128, D], F32) for i in range(2)]
        h1p = c.sbuf("h1", [128, D], F32)
        gbcp = c.sbuf("gbc", [128, D], F32)
        xr = [c.sbuf(f"xr{i}", [128, 512], F32) for i in range(2)]
        mod_step(c, nc, ccol_d, modw_d, modb_d, g_d, ps, outs, gbcp, xr, wsl)
        ob = {n: c.buf("o_" + n) for n in ("QT", "KT", "SG", "V", "QI", "KI", "WI", "GB")}
        c.outbufs += list(ob.values())
        c.dma("sp", GB_d, outs["gate"][0][:], [outs["gate"][1]], ob["GB"])
        cs, csb = rope_tables(c, pos_d, invf_d)
        if stop == "rope":
            dbg_d = dt("dbg", [128, NB * 48], F32, kind="ExternalOutput").ap()
            c.dma("sp", dbg_d, cs[:].rearrange("p a b f -> p (a b f)"), [csb], c.buf("dbg"))
            c.emit()
            return nc
        hT, hTb = c.sbuf("hT", [128, 16, NB * 128], BF16)
        hbs = [c.sbuf(f"hb{i}", [128, D], BF16) for i in range(2)]
        tmp = {"sq": c.sbuf("sq", [128, D], BF16), "ss": c.sbuf("ss", [128, 1], F32),
               "rs": c.sbuf("rs", [128, 1], F32), "h1": h1p}
        for rb in range(NB):
            xt, xb = xin[rb % 2]
            c.dma("sp", xt[:], x_d[rb], [], xb)
            ht, hb = hbs[rb % 2]
            norm_rows(c, xt[:], xb, outs, tmp, ht[:], hb)
            for q in range(4):
                pT, pb = pTs[q % 2]
                for f in range(4):
                    fc = q * 4 + f
                    c.op("pe", "transpose", [hb, idb], [pb], pT[:, f * 128:(f + 1) * 128],
                         ht[:, fc * 128:(fc + 1) * 128], idt[:], signal=(f == 3))
                dst = hT[:, q * 4:(q + 1) * 4, rb * 128:(rb + 1) * 128]
                srcv = pT[:, 0:512].rearrange("p (f t) -> p f t", f=4)
                if q % 2 == 0:
                    c.op("act", "activation", [pb], [hTb], out=dst, in_=srcv, func=AF.Copy)
                else:
                    c.op("dve", "tensor_copy", [pb], [hTb], out=dst, in_=srcv)
        cexp = []
        for nm, nh_e, half_e, f0_e in (("cq", 4, 16, 0), ("ci", 8, 8, 16)):
            ce, ceb = c.sbuf(nm, [128, NB, 2, 64], F32)
            for b in range(NB):
                for t_ in range(2):
                    c.op("dve", "tensor_copy", [csb], [ceb], out=ce[:, b, t_, :].rearrange("p (h d) -> p h d", h=nh_e),
                         in_=cs[:, b, t_, f0_e:f0_e + half_e].unsqueeze(1).to_broadcast([128, nh_e, half_e]))
            cexp.append((ce, ceb))
        winv = win_d.rearrange("(kc p) n -> p kc n", p=128)
        tms = [c.sbuf(f"tm{i}", [128, 512], BF16) for i in range(2)]
        xfs = [c.sbuf(f"xf{i}", [128, 512], F32) for i in range(2)]
        stg = [c.sbuf(f"stg{i}", [128, 512], BF16) for i in range(2)]
        rt = [c.sbuf(f"rt{i}", [128, 64], F32) for i in range(4)]
        wif = [c.sbuf(f"wif{i}", [128, 16], F32) for i in range(2)]
        chunks = list(range(19)) if stop is None else stop
        it = 0
        for ch in chunks:
            c0 = ch * 512
            ncol = 512 if ch < 18 else ATT_IN - 18 * 512
            Wt, Wb = wsl[ch % 3]
            W = Wt[:, 0:16 * ncol].rearrange("p (k n) -> p k n", k=16)
            c.dma("pool", W, winv[:, :, c0:c0 + ncol], [], Wb)
            kind = ("q", "k", "v", "g", "qi")[ch // 4] if ch < 18 else "ki"
            for b in range(NB):
                pt, pb = ps[it % 4]
                tm, tmb = tms[it % 2]
                sg_, sgb = stg[it % 2]
                pT, pTb_ = pTs[it % 2]
                it += 1
                for kc in range(16):
                    c.op("pe", "matmul", [hTb, Wb], [pb], pt[:, 0:ncol], hT[:, kc, b * 128:(b + 1) * 128], W[:, kc, :],
                         start=(kc == 0), stop=(kc == 15), signal=(kc == 15))
                tsl = slice(b * 128, (b + 1) * 128)
                if kind == "v":
                    c.op("act", "activation", [pb], [tmb], out=tm[:], in_=pt[:], func=AF.Copy)
                    c.dma("sp", V_d[tsl, (ch - 8) * 512:(ch - 7) * 512], tm[:], [tmb], ob["V"], nowaw=True, semof=tmb)
                    continue
                if kind == "g":
                    c.op("act", "activation", [pb], [tmb], out=tm[:], in_=pt[:], func=AF.Silu)
                else:
                    if kind in ("q", "k"):
                        nh_, hd, half, f0 = 4, 128, 16, 0
                    elif kind == "qi":
                        nh_, hd, half, f0 = 8, 64, 8, 16
                    else:
                        nh_, hd, half, f0 = 1, 64, 8, 16
                    wcol = nh_ * hd
                    xf_, xfb_ = xfs[it % 2]
                    c.op("act", "activation", [pb], [xfb_], out=xf_[:, 0:wcol], in_=pt[:, 0:wcol], func=AF.Copy)
                    c.op("act", "activation", [pb], [tmb], out=tm[:, 0:wcol], in_=pt[:, 0:wcol], func=AF.Copy)
                    pv = xf_[:, 0:wcol].rearrange("p (h d) -> p h d", h=nh_)
                    tv = tm[:, 0:wcol].rearrange("p (h d) -> p h d", h=nh_)
                    rsb_ = xfb_
                    ceb_ = csb
                    if kind == "ki":
                        cosb = cs[:, b, 0, f0:f0 + half].unsqueeze(1)
                        sinb = cs[:, b, 1, f0:f0 + half].unsqueeze(1)
                    else:
                        ce, ceb_ = cexp[0] if kind in ("q", "k") else cexp[1]
                        cosb = ce[:, b, 0, :].rearrange("p (h d) -> p h d", h=nh_)
                        sinb = ce[:, b, 1, :].rearrange("p (h d) -> p h d", h=nh_)
                    x1 = pv[:, :, 0:half]
                    x2 = pv[:, :, half:2 * half]
                    tt = [(rt[i][0][:, 0:nh_ * half].rearrange("p (h d) -> p h d", h=nh_), rt[i][1]) for i in range(4)]
                    c.op("dve", "tensor_tensor", [rsb_, ceb_], [tt[0][1]], out=tt[0][0], in0=x1, in1=cosb, op=ALU.mult,
                         signal=False)
                    c.op("dve", "tensor_tensor", [rsb_, ceb_], [tt[1][1]], out=tt[1][0], in0=x2, in1=sinb, op=ALU.mult,
                         signal=False)
                    c.op("dve", "tensor_tensor", [rsb_, ceb_], [tt[2][1]], out=tt[2][0], in0=x2, in1=cosb, op=ALU.mult,
                         signal=False)
                    c.op("dve", "tensor_tensor", [rsb_, ceb_], [tt[3][1]], out=tt[3][0], in0=x1, in1=sinb, op=ALU.mult)
                    c.op("dve", "tensor_tensor", [tt[0][1], tt[1][1]], [tmb], out=tv[:, :, 0:half], in0=tt[0][0],
                         in1=tt[1][0], op=ALU.subtract, signal=False)
                    c.op("dve", "tensor_tensor", [tt[2][1], tt[3][1]], [tmb], out=tv[:, :, half:2 * half], in0=tt[2][0],
                         in1=tt[3][0], op=ALU.add)
                    if kind == "ki":
                        wt_, wb_ = wif[b % 2]
                        c.op("act", "activation", [pb], [wb_], out=wt_[:], in_=pt[:, 64:80], func=AF.Copy)
                        c.dma("sp", WI_d[:, b, :], wt_[:], [wb_], ob["WI"], nowaw=True, semof=wb_)
                if kind == "ki":
                    c.op("pe", "transpose", [tmb, idb], [pTb_], pT[:, 0:128], tm[:, 0:128], idt[:])
                    c.op("dve", "tensor_copy", [pTb_], [sgb], out=sg_[0:64, 0:128], in_=pT[0:64, 0:128])
                    c.dma("sp", KI_d[:, tsl], sg_[0:64, 0:128], [sgb], ob["KI"], nowaw=True, semof=sgb)
                    continue
                for f in range(4):
                    c.op("pe", "transpose", [tmb, idb], [pTb_], pT[:, f * 128:(f + 1) * 128],
                         tm[:, f * 128:(f + 1) * 128], idt[:], signal=(f == 3))
                c.op("dve", "tensor_copy", [pTb_], [sgb], out=sg_[:], in_=pT[:, 0:512])
                dd, key, hc = {"q": (QT_d, "QT", ch), "k": (KT_d, "KT", ch - 4), "g": (SG_d, "SG", ch - 12),
                               "qi": (QI_d, "QI", ch - 16)}[kind]
                dst = dd[hc * 4:(hc + 1) * 4, :, tsl].rearrange("h d t -> d h t")
                c.dma("sp", dst, sg_[:].rearrange("p (h t) -> p h t", h=4), [sgb], ob[key], nowaw=True, semof=sgb)
        c.emit()
    return nc


NIT = 20
TOPK = 256
BIG = 1.0e30


def mt_base(kb):
    J = kb // 8
    return sum(8 * (8 - jj) for jj in range(J)) + (kb - 8 * J) * (8 - J)


def build_a2(stop=None):
    nc = bass.Bass("TRN2", target_bir_lowering=False)
    dt = nc.dram_tensor
    S = NB * 128 * NCORE
    x_d = dt("xs", [NB, 128, D], F32, kind="ExternalInput").ap()
    GB_d = dt("GBC", [128, D], F32, kind="ExternalInput").ap()
    QT_d = dt("QT", [NH, 128, NB * 128], BF16, kind="ExternalInput").ap()
    SG_d = dt("SG", [NH, 128, NB * 128], BF16, kind="ExternalInput").ap()
    QI_d = dt("QIT", [8, 128, NB * 128], BF16, kind="ExternalInput").ap()
    WI_d = dt("WI", [128, NB, 16], F32, kind="ExternalInput").ap()
    KT_d = dt("KTg", [NH, 128, S], BF16, kind="ExternalInput").ap()
    V_d = dt("Vg", [S, D], BF16, kind="ExternalInput").ap()
    KI_d = dt("KITg", [64, S], BF16, kind="ExternalInput").ap()
    trel_d = dt("trel", [128, 1], F32, kind="ExternalInput").ap()
    wout_d = dt("wout", [D, D], F32, kind="ExternalInput").ap()
    xo_d = dt("xo", [NB, 128, D], F32, kind="ExternalOutput").ap()
    with ExitStack() as es:
        c = Ctx(nc, es)
        K = make_consts(c, nc)
        idt, idb = K["ident"]
        ps = [c.psum(f"ps{i}", [128, 512], F32) for i in range(8)]
        pTs = [(ps[6][0][:].bitcast(BF16), ps[6][1]), (ps[7][0][:].bitcast(BF16), ps[7][1])]
        mT, mTb = c.sbuf("mT", [128, 288, 128], BF16)
        ones, onesb = c.sbuf("ones", [128, 128], BF16)
        c.op("pool", "memset", [], [onesb], ones[:], 1.0)
        with ExitStack() as es2:
            c.es = es2
            ki2, ki2b = c.sbuf("ki2", [128, S], BF16)
            c.dma("sp", ki2[0:64, :], KI_d, [], ki2b)
            c.dma("sp", ki2[64:128, :], KI_d, [], ki2b)
            qit, qitb = c.sbuf("qit", [128, 8, NB * 128], BF16)
            c.dma("sp", qit[:], QI_d.rearrange("c p t -> p c t"), [], qitb)
            wi, wib = c.sbuf("wi", [128, NB, 16], F32)
            c.dma("sp", wi[:], WI_d, [], wib)
            trel, trelb = c.sbuf("trel", [128, 1], F32)
            c.dma("sp", trel[:], trel_d, [], trelb)
            ioi, ioib = c.sbuf("ioi", [128, 1024], I32)
            cm, cmb = c.sbuf("cm", [128, 1024], F32)
            nbias, nbb = c.sbuf("nbias", [128, 1024], F32)
            c.op("pool", "iota", [], [ioib], ioi[:], pattern=[[1, 1024]], base=0, channel_multiplier=0)
            c.op("dve", "tensor_copy", [ioib], [cmb], out=cm[:], in_=ioi[:])
            c.op("dve", "tensor_scalar", [cmb, trelb], [cmb], out=cm[:], in0=cm[:], scalar1=trel[:, 0:1], scalar2=None,
                 op0=ALU.is_le)
            c.op("dve", "tensor_scalar", [cmb], [nbb], out=nbias[:], in0=cm[:], scalar1=-1.0, scalar2=BIG,
                 op0=ALU.add, op1=ALU.mult)
            p2, p2b = c.sbuf("pow2", [128, NIT + 2], F32)
            for k in range(NIT + 2):
                c.op("pool", "memset", [], [p2b], p2[:, k:k + 1], float(2.0 ** (-k)))
            Sc, Scb = c.sbuf("Sc", [128, S], F32)
            mk, mkb = c.sbuf("mk", [128, S], BF16)
            rr = [c.sbuf(f"rr{i}", [128, 512], F32) for i in range(4)]
            accB, accBb = c.sbuf("accB", [128, 512], F32)
            sm = {n: c.sbuf("bs_" + n, [128, 1], F32) for n in ("mn", "mx", "W", "cand", "cnt", "u", "thr")}
            steps, stepsb = c.sbuf("steps", [128, NIT + 2], F32)
            it = 0
            for j in range(NB):
                n = 1024 * (j + 1)
                tsl = slice(j * 128, (j + 1) * 128)
                for kt in range(2 * (j + 1)):
                    ksl = slice(kt * 512, (kt + 1) * 512)
                    for h in range(16):
                        par = h % 2
                        psl = slice(par * 64, (par + 1) * 64)
                        pt, pb = ps[it % 6]
                        rt_, rb_ = rr[it % 4]
                        it += 1
                        c.op("pe", "matmul", [qitb, ki2b], [pb], pt[:], qit[psl, h // 2, tsl], ki2[psl, ksl],
                             start=True, stop=True)
                        c.op("act", "activation", [pb], [rb_], out=rt_[:], in_=pt[:], func=AF.Relu)
                        if par == 0:
                            eng, at, ab = "dve", Sc[:, ksl], Scb
                        else:
                            eng, at, ab = "pool", accB[:], accBb
                        if h < 2:
                            c.op(eng, "tensor_scalar", [rb_, wib], [ab], out=at, in0=rt_[:], scalar1=wi[:, j, h:h + 1],
                                 scalar2=None, op0=ALU.mult)
                        elif eng == "dve":
                            c.op(eng, "scalar_tensor_tensor", [rb_, wib, ab], [ab], out=at, in0=rt_[:],
                                 scalar=wi[:, j, h:h + 1], in1=at, op0=ALU.mult, op1=ALU.add)
                        else:
                            c.op(eng, "tensor_scalar", [rb_, wib], [rb_], out=rt_[:], in0=rt_[:],
                                 scalar1=wi[:, j, h:h + 1], scalar2=None, op0=ALU.mult)
                            c.op(eng, "tensor_tensor", [rb_, ab], [ab], out=at, in0=at, in1=rt_[:], op=ALU.add)
                    c.op("dve", "tensor_tensor", [Scb, accBb], [Scb], out=Sc[:, ksl], in0=Sc[:, ksl], in1=accB[:],
                         op=ALU.add)
                mn, mnb = sm["mn"]; mx, mxb = sm["mx"]; W_, Wb_ = sm["W"]; cand, candb = sm["cand"]
                cnt, cntb = sm["cnt"]; u_, ub_ = sm["u"]; thr, thrb = sm["thr"]
                c.op("dve", "tensor_reduce", [Scb], [mnb], out=mn[:], in_=Sc[:, 0:n], axis=AX.X, op=ALU.min)
                c.op("dve", "tensor_reduce", [Scb], [mxb], out=mx[:], in_=Sc[:, 0:n], axis=AX.X, op=ALU.max)
                c.op("dve", "tensor_tensor", [mxb, mnb], [Wb_], out=W_[:], in0=mx[:], in1=mn[:], op=ALU.subtract)
                c.op("dve", "tensor_scalar", [Wb_], [stepsb], out=steps[:], in0=p2[:], scalar1=W_[:, 0:1], scalar2=None,
                     op0=ALU.mult)
                c.op("dve", "tensor_tensor", [mnb, stepsb], [candb], out=cand[:], in0=mn[:], in1=steps[:, 1:2],
                     op=ALU.add)
                lsl = slice(n - 1024, n)
                c.op("dve", "tensor_tensor", [Scb, cmb], [Scb], out=Sc[:, lsl], in0=Sc[:, lsl], in1=cm[:], op=ALU.mult)
                c.op("dve", "tensor_tensor", [Scb, nbb], [Scb], out=Sc[:, lsl], in0=Sc[:, lsl], in1=nbias[:], op=ALU.add)
                for k in range(1, NIT + 1):
                    c.op("dve", "tensor_scalar", [Scb, candb], [mkb, cntb], out=mk[:, 0:n], in0=Sc[:, 0:n],
                         scalar1=cand[:, 0:1], scalar2=None, op0=ALU.is_ge, op1=ALU.add, accum_out=cnt[:])
                    c.op("dve", "tensor_scalar", [cntb], [ub_], out=u_[:], in0=cnt[:], scalar1=float(TOPK) - 0.5,
                         scalar2=0.5, op0=ALU.is_ge, op1=ALU.subtract)
                    c.op("dve", "scalar_tensor_tensor", [ub_, stepsb, candb], [candb], out=cand[:], in0=u_[:],
                         scalar=steps[:, k:k + 1], in1=cand[:], op0=ALU.mult, op1=ALU.add)
                c.op("dve", "scalar_tensor_tensor", [stepsb, candb], [thrb], out=thr[:], in0=steps[:, NIT + 1:NIT + 2],
                     scalar=-2.0, in1=cand[:], op0=ALU.mult, op1=ALU.add)
                c.op("dve", "tensor_scalar", [Scb, thrb], [mkb], out=mk[:, 0:n], in0=Sc[:, 0:n], scalar1=thr[:, 0:1],
                     scalar2=None, op0=ALU.is_ge)
                for g4 in range(2 * (j + 1)):
                    kb0 = g4 * 4
                    J = kb0 // 8
                    pT, pTb_ = pTs[g4 % 2]
                    for f in range(4):
                        kb = kb0 + f
                        c.op("pe", "transpose", [mkb, idb], [pTb_], pT[:, f * 128:(f + 1) * 128],
                             mk[:, kb * 128:(kb + 1) * 128], idt[:], signal=(f == 3))
                    b0 = mt_base(8 * J)
                    dst = mT[:, b0:b0 + 8 * (8 - J), :].rearrange("p (kb jj) t -> p kb jj t", jj=8 - J)[
                        :, kb0 - 8 * J:kb0 - 8 * J + 4, j - J, :]
                    src = pT[:, 0:512].rearrange("p (f t) -> p f t", f=4)
                    if g4 % 2 == 0:
                        c.op("act", "activation", [pTb_], [mTb], out=dst, in_=src, func=AF.Copy)
                    else:
                        c.op("pool", "tensor_copy", [pTb_], [mTb], out=dst, in_=src) if False else \
                            c.op("dve", "tensor_copy", [pTb_], [mTb], out=dst, in_=src)
            c.barrier()
        c.es = es
        kts = [c.sbuf(f"kts{i}", [128, S], BF16) for i in range(1)]
        vts = [c.sbuf(f"vts{i}", [128, 64, 128], BF16) for i in range(1)]
        qts = [c.sbuf(f"qts{i}", [128, NB * 128], BF16) for i in range(2)]
        sgs = [c.sbuf(f"sgs{i}", [128, NB * 128], BF16) for i in range(2)]
        pex = [c.sbuf(f"pex{i}", [128, 512], BF16) for i in range(3)]
        pms = [c.sbuf(f"pms{i}", [128, 512], BF16) for i in range(3)]
        ogT, ogTb = c.sbuf("ogT", [128, NH, NB * 128], BF16)
        rl, rlb = c.sbuf("rl", [128, 512], F32)
        on_, onb = c.sbuf("on", [128, 512], F32)
        scale = float(128 ** -0.5)
        it = 0
        hh = 0
        for h in range(NH):
            ktt, ktb = kts[0]
            vtt, vtb = vts[0]
            qtt, qtb = qts[h % 2]
            sgt, sgb = sgs[h % 2]
            for q4 in range(4):
                c.dma("sp", ktt[:, q4 * 2048:(q4 + 1) * 2048], KT_d[h, :, q4 * 2048:(q4 + 1) * 2048], [], ktb,
                      nowaw=(q4 > 0))
                c.dma("sp", vtt[:, q4 * 16:(q4 + 1) * 16, :],
                      V_d[q4 * 2048:(q4 + 1) * 2048, h * 128:(h + 1) * 128].rearrange("(sb p) d -> p sb d", p=128), [],
                      vtb, nowaw=(q4 > 0))
            c.dma("sp", qtt[:], QT_d[h], [], qtb)
            c.dma("sp", sgt[:], SG_d[h], [], sgb)
            for half in range(2):
                jlo = 4 * half
                po, pob = ps[4 + 2 * (hh % 2)]
                pl, plb = ps[5 + 2 * (hh % 2)]
                hh += 1
                nkb = 8 * (jlo + 4)
                for kb in range(nkb):
                    J = kb // 8
                    j0 = max(J, jlo)
                    nj = jlo + 4 - j0
                    N = 128 * nj
                    t0 = 128 * j0
                    pL, pLb = ps[it % 4]
                    pe_, peb = pex[it % 3]
                    pm_, pmb = pms[it % 3]
                    it += 1
                    c.op("pe", "matmul", [ktb, qtb], [pLb], pL[:, 0:N], ktt[:, kb * 128:(kb + 1) * 128], qtt[:, t0:t0 + N],
                         start=True, stop=True)
                    c.op("act", "activation", [pLb], [peb], out=pe_[:, 0:N], in_=pL[:, 0:N], func=AF.Exp, scale=scale)
                    mb0 = mt_base(kb) + (j0 - J)
                    eng = "dve" if it % 2 == 0 else "pool"
                    c.op(eng, "tensor_tensor", [peb, mTb], [pmb], out=pm_[:, 0:N], in0=pe_[:, 0:N],
                         in1=mT[:, mb0:mb0 + nj, :].rearrange("p a t -> p (a t)"), op=ALU.mult)
                    osl = slice((j0 - jlo) * 128, 512)
                    st_, sp_ = (kb == 0), (kb == nkb - 1)
                    c.op("pe", "matmul", [vtb, pmb], [pob], po[:, osl], vtt[:, kb, :], pm_[:, 0:N],
                         start=st_, stop=sp_, signal=False)
                    c.op("pe", "matmul", [onesb, pmb], [plb], pl[:, osl], ones[:], pm_[:, 0:N],
                         start=st_, stop=sp_, signal=True)
                hsl = slice(half * 512, (half + 1) * 512)
                c.op("dve", "reciprocal", [plb], [rlb], out=rl[:], in_=pl[:])
                c.op("dve", "tensor_tensor", [pob, rlb], [onb], out=on_[:], in0=po[:], in1=rl[:], op=ALU.mult)
                c.op("pool", "tensor_tensor", [onb, sgb], [ogTb], out=ogT[:, h, hsl], in0=on_[:], in1=sgt[:, hsl],
                     op=ALU.mult)
        gbc, gbcb = c.sbuf("gbc", [128, D], F32)
        c.dma("sp", gbc[:], GB_d, [], gbcb)
        wsl = [c.sbuf(f"wsl{i}", [128, 16, 512], BF16) for i in range(2)]
        xo = [c.sbuf(f"xo{i}", [128, 512], F32) for i in range(2)]
        xr = [c.sbuf(f"xr{i}", [128, 512], F32) for i in range(2)]
        xod = c.buf("xo_d")
        woutv = wout_d.rearrange("(kc p) m -> p kc m", p=128)
        it = 0
        for mc in range(4):
            Wt, Wb = wsl[mc % 2]
            msl = slice(mc * 512, (mc + 1) * 512)
            c.dma("pool", Wt[:], woutv[:, :, msl], [], Wb)
            for b in range(NB):
                po, pob = ps[it % 4]
                xrt, xrb = xr[it % 2]
                xot, xob = xo[it % 2]
                it += 1
                for hc in range(16):
                    c.op("pe", "matmul", [ogTb, Wb], [pob], po[:], ogT[:, hc, b * 128:(b + 1) * 128], Wt[:, hc, :],
                         start=(hc == 0), stop=(hc == 15), signal=(hc == 15))
                c.dma("sp", xrt[:], x_d[b, :, msl], [], xrb)
                c.op("dve", "tensor_tensor", [pob, gbcb], [xob], out=xot[:], in0=po[:], in1=gbc[:, msl], op=ALU.mult)
                c.op("pool", "tensor_tensor", [xob, xrb], [xob], out=xot[:], in0=xot[:], in1=xrt[:], op=ALU.add)
                c.dma("sp", xo_d[b, :, msl], xot[:], [xob], xod, nowaw=True, semof=xob)
        c.emit()
    return nc


def build_final():
    nc = bass.Bass("TRN2", target_bir_lowering=False)
    dt = nc.dram_tensor
    x_d = dt("xs", [NB, 128, D], F32, kind="ExternalInput").ap()
    g_d = dt("g", [1, D], F32, kind="ExternalInput").ap()
    xo_d = dt("xo", [NB, 128, D], F32, kind="ExternalOutput").ap()
    with ExitStack() as es:
        c = Ctx(nc, es)
        gbc, gbcb = c.sbuf("gbc", [128, D], F32)
        c.dma("sp", gbc[:], g_d.partition_broadcast(128), [], gbcb)
        xin = [c.sbuf(f"xin{i}", [128, D], F32) for i in range(2)]
        xo = [c.sbuf(f"xo{i}", [128, D], F32) for i in range(2)]
        sq, sqb = c.sbuf("sq", [128, D], BF16)
        ss, ssb = c.sbuf("ss", [128, 1], F32)
        rs, rsb = c.sbuf("rs", [128, 1], F32)
        xod = c.buf("xo_d")
        for b in range(NB):
            xt, xb = xin[b % 2]
            ot, ob = xo[b % 2]
            c.dma("sp", xt[:], x_d[b], [], xb)
            c.op("act", "activation", [xb], [sqb, ssb], out=sq[:], in_=xt[:], func=AF.Square, accum_out=ss[:])
            c.op("dve", "tensor_scalar", [ssb], [rsb], out=rs[:], in0=ss[:], scalar1=1.0 / D, scalar2=EPS,
                 op0=ALU.mult, op1=ALU.add)
            c.op("act", "activation", [rsb], [rsb], out=rs[:], in_=rs[:], func=AF.Sqrt)
            c.op("dve", "reciprocal", [rsb], [rsb], out=rs[:], in_=rs[:])
            c.op("dve", "scalar_tensor_tensor", [xb, rsb, gbcb], [ob], out=ot[:], in0=xt[:], scalar=rs[:, 0:1],
                 in1=gbc[:], op0=ALU.mult, op1=ALU.mult)
            c.dma("sp", xo_d[b], ot[:], [ob], xod, nowaw=True, semof=ob)
        c.emit()
    return nc


def _gb(i, j):
    return 8 * j + i


def _shard_x(x, i):
    return np.ascontiguousarray(np.stack([x[128 * _gb(i, j):128 * _gb(i, j) + 128] for j in range(NB)]))


def _unshard_x(shards):
    out = np.zeros((NB * NCORE * 128, D), np.float32)
    for i in range(NCORE):
        for j in range(NB):
            out[128 * _gb(i, j):128 * _gb(i, j) + 128] = shards[i][j]
    return out


def _halo(x, i):
    xh = np.zeros((128, D), np.float32)
    tok = np.zeros((1, NCOL), np.float32)
    for j in range(NB):
        s = 128 * _gb(i, j)
        if s >= 16:
            xh[j * 16:(j + 1) * 16] = x[s - 16:s]
        tok[0, j * HC:(j + 1) * HC] = np.arange(s - 16, s + 128)
    return xh, tok


def _invf_row():
    a = 500000.0 ** (-np.arange(16, dtype=np.float32) * 2.0 / 32)
    b = 500000.0 ** (-np.arange(8, dtype=np.float32) * 2.0 / 16)
    return np.concatenate([a, b]).astype(np.float32)[None]


def _run(nc, maps):
    return run_bass_kernel_spmd(nc, maps, core_ids=list(range(NCORE))).results


def kernel(x, c, positions, norm_g, mod_w, mod_b, pool_w_in, pool_w_grp, pool_scale, pool_w_out,
           attn_w_in, attn_w_out, final_g):
    f32 = np.float32
    xc = np.ascontiguousarray(np.asarray(x, f32)[0])
    ccol = np.ascontiguousarray(np.asarray(c, f32)[0].reshape(16, 128).T)
    pos = np.asarray(positions)[0].astype(np.int32)
    ncs = {}

    def prog(name, fn):
        if name not in ncs:
            ncs[name] = fn()
        return ncs[name]

    for li in range(4):
        j2 = li // 2
        common = dict(ccol=ccol, modw=np.ascontiguousarray(mod_w[li], f32), modb=np.asarray(mod_b[li], f32)[None],
                      g=np.asarray(norm_g[li], f32)[None])
        if li % 2 == 0:
            maps = []
            lscol = np.ascontiguousarray(np.asarray(pool_scale[j2], f32).reshape(16, 128).T)
            for i in range(NCORE):
                xh, tok = _halo(xc, i)
                maps.append(dict(xs=_shard_x(xc, i), xh=xh, tokrow=tok, win=np.asarray(pool_w_in[j2], f32),
                                 wgrp=np.asarray(pool_w_grp[j2], f32), lscol=lscol,
                                 wout=np.asarray(pool_w_out[j2], f32), **common))
            res = _run(prog("pool", build_pool), maps)
            xc = _unshard_x([r["xo"] for r in res])
        else:
            maps = []
            for i in range(NCORE):
                posc = np.ascontiguousarray(
                    np.stack([pos[128 * _gb(i, j):128 * _gb(i, j) + 128] for j in range(NB)], axis=1))
                maps.append(dict(xs=_shard_x(xc, i), win=np.asarray(attn_w_in[j2], f32), posc=posc,
                                 invf=_invf_row(), **common))
            r1 = _run(prog("a1", build_a1), maps)
            S = NB * NCORE * 128
            bf = ml_dtypes.bfloat16
            KTg = np.zeros((NH, 128, S), bf)
            Vg = np.zeros((S, D), bf)
            KITg = np.zeros((64, S), bf)
            for i in range(NCORE):
                for j in range(NB):
                    gs = slice(128 * _gb(i, j), 128 * _gb(i, j) + 128)
                    ls = slice(128 * j, 128 * j + 128)
                    KTg[:, :, gs] = r1[i]["KT"][:, :, ls]
                    Vg[gs] = r1[i]["V"][ls]
                    KITg[:, gs] = r1[i]["KIT"][:, ls]
            maps = []
            for i in range(NCORE):
                maps.append(dict(xs=_shard_x(xc, i), GBC=r1[i]["GBC"], QT=r1[i]["QT"], SG=r1[i]["SG"], QIT=r1[i]["QIT"],
                                 WI=r1[i]["WI"], KTg=KTg, Vg=Vg, KITg=KITg,
                                 trel=(128 * i + np.arange(128, dtype=f32))[:, None],
                                 wout=np.asarray(attn_w_out[j2], f32)))
            r2 = _run(prog("a2", build_a2), maps)
            xc = _unshard_x([r["xo"] for r in r2])
    maps = [dict(xs=_shard_x(xc, i), g=np.asarray(final_g, f32)[None]) for i in range(NCORE)]
    rf = _run(prog("final", build_final), maps)
    out = _unshard_x([r["xo"] for r in rf])
    return out[None].astype(np.float32)
```

```python
import numpy as np
import ml_dtypes
from contextlib import ExitStack
import concourse.bass as bass
import concourse.mybir as mybir
from concourse.bass_utils import run_bass_kernel_spmd

F32 = mybir.dt.float32
BF16 = mybir.dt.bfloat16
I32 = mybir.dt.int32
ALU = mybir.AluOpType
AF = mybir.ActivationFunctionType
AX = mybir.AxisListType


class Buf:
    __slots__ = ("name", "lw", "rd", "dsem", "dval")

    def __init__(self, name):
        self.name = name
        self.lw = None
        self.rd = []
        self.dsem = None
        self.dval = 0


class Eng:
    def __init__(self, name):
        self.name = name
        self.sem = None
        self.count = 0
        self.ops = []
        self.waited = {}


class Ctx:
    def __init__(self, nc, es):
        self.nc = nc
        self.es = es
        self.es0 = es
        self.eng = {n: Eng(n) for n in ("pe", "act", "dve", "pool", "sp")}
        for n in ("pe", "act", "dve", "pool"):
            self.eng[n].sem = es.enter_context(nc.semaphore("s_" + n))
        self.nsem = 4
        self.outbufs = []
        self.dmabufs = []

    def buf(self, name):
        return Buf(name)

    def sbuf(self, name, shape, dt):
        t = self.es.enter_context(self.nc.sbuf_tensor("sb_" + name, list(shape), dt))
        return t, Buf(name)

    def psum(self, name, shape, dt):
        t = self.es.enter_context(self.nc.psum_tensor("pp_" + name, list(shape), dt))
        return t, Buf(name)

    def _wait(self, C, dep):
        if dep[0] == "eng":
            E, idx = dep[1], dep[2]
            assert E.count >= idx, f"dep on unsignaled instr {E.name} {idx} {E.count}"
            sem, val = E.sem, idx
        else:
            sem, val = dep[1], dep[2]
        k = id(sem)
        if C.waited.get(k, 0) >= val:
            return
        C.waited[k] = val
        C.ops.append(("wait", sem, val))

    def _deps(self, C, reads, writes, is_dma):
        deps = []
        for r in reads:
            if r.lw is not None:
                d = r.lw
                if d[0] == "eng" and d[1] is C and C.name == "pe":
                    continue
                deps.append(d)
        for w in writes:
            if w.lw is not None:
                d = w.lw
                if not (d[0] == "eng" and d[1] is C and not is_dma):
                    deps.append(d)
            for d in w.rd:
                if not (d[0] == "eng" and d[1] is C and not is_dma):
                    deps.append(d)
        for d in deps:
            self._wait(C, d)

    def op(self, eng, meth, reads, writes, *args, signal=True, **kw):
        C = self.eng[eng]
        self._deps(C, reads, writes, False)
        if signal:
            C.count += 1
            idx = C.count
        else:
            idx = C.count + 1
        C.ops.append(("inst", meth, args, kw, signal))
        rec = ("eng", C, idx)
        for r in reads:
            r.rd.append(rec)
        for w in writes:
            w.lw = rec
            w.rd = []

    def dma(self, q, out, in_, reads, wbuf, nowaw=False, semof=None, **kw):
        C = self.eng[q]
        sb = semof if semof is not None else wbuf
        if nowaw:
            sv = wbuf.lw
            wbuf.lw = None
            self._deps(C, reads, [wbuf], True)
            wbuf.lw = sv
        else:
            self._deps(C, reads, [wbuf], True)
        if sb.dsem is None:
            sb.dsem = self.es0.enter_context(self.nc.semaphore("d_" + sb.name))
            self.nsem += 1
            self.dmabufs.append(sb)
        sb.dval += 16
        C.ops.append(("dma", out, in_, kw, sb.dsem))
        rec = ("dma", sb.dsem, sb.dval)
        for r in reads:
            r.rd.append(rec)
        wbuf.lw = rec
        wbuf.rd = []

    def barrier(self):
        comp = [self.eng[n] for n in ("pe", "act", "dve", "pool")]
        for C in self.eng.values():
            for E in comp:
                if E is not C and E.count > 0:
                    self._wait(C, ("eng", E, E.count))
            for b in self.dmabufs:
                self._wait(C, ("dma", b.dsem, b.dval))

    def emit(self):
        nc = self.nc
        C = self.eng["sp"]
        for b in self.dmabufs:
            self._wait(C, ("dma", b.dsem, b.dval))
        hwmap = {"pe": "tensor", "act": "scalar", "dve": "vector", "pool": "gpsimd", "sp": "sync"}
        with nc.Block() as block:
            for n, E in self.eng.items():
                def body(hw, E=E):
                    for o in E.ops:
                        if o[0] == "wait":
                            hw.wait_ge(o[1], o[2])
                        elif o[0] == "inst":
                            ins = getattr(hw, o[1])(*o[2], **o[3])
                            if o[4]:
                                ins.then_inc(E.sem, 1)
                        else:
                            hw.dma_start(out=o[1], in_=o[2], **o[3]).then_inc(o[4], 16)
                getattr(block, hwmap[n])(body)


D = 2048
NB = 8
NCORE = 8
HC = 144
NCOL = NB * HC
EPS = 1e-6
WINS = (2, 4, 8, 16)


def make_consts(c, nc):
    K = {}
    idt, idb = c.sbuf("ident", [128, 128], BF16)
    c.op("pool", "memset", [], [idb], idt[:], 0.0)
    c.op("pool", "affine_select", [idb], [idb], out=idt[:], in_=idt[:], pattern=[[-1, 128]],
         compare_op=ALU.not_equal, fill=1.0, base=0, channel_multiplier=1)
    K["ident"] = (idt, idb)
    return K


def mod_step(c, nc, ccol_d, modw_d, modb_d, g_d, ps, outs, gbc_pair, mbb, ws):
    ccol, ccb = c.sbuf("ccol", [128, 16], F32)
    cond, condb = c.sbuf("cond", [128, 16], F32)
    crep, crepb = c.sbuf("crep", [128, 16, 128], BF16)
    gbc, gbcb = gbc_pair
    c.dma("sp", ccol[:], ccol_d, [], ccb)
    c.op("act", "activation", [ccb], [condb], out=cond[:], in_=ccol[:], func=AF.Silu)
    c.op("dve", "tensor_copy", [condb], [crepb], out=crep[:],
         in_=cond[:].unsqueeze(2).to_broadcast([128, 16, 128]))
    c.dma("sp", gbc[:], g_d.partition_broadcast(128), [], gbcb)
    mw = modw_d.rearrange("(kc p) n -> p kc n", p=128)
    names = ["shift", "geff", "gate"]
    for nch in range(12):
        wt, wb = ws[nch % 2]
        bt, bb = mbb[nch % 2]
        wt = wt[:].rearrange("p (k n) -> p k n", k=16)
        c.dma("pool", wt, mw[:, :, nch * 512:(nch + 1) * 512], [], wb)
        c.dma("sp", bt[:], modb_d[:, nch * 512:(nch + 1) * 512].partition_broadcast(128), [], bb)
        pt, pb = ps[nch % 2]
        for kc in range(16):
            c.op("pe", "matmul", [crepb, wb], [pb], pt[:], crep[:, kc, :], wt[:, kc, :],
                 start=(kc == 0), stop=(kc == 15), signal=(kc == 15))
        ot, ob = outs[names[nch // 4]]
        sl = slice((nch % 4) * 512, (nch % 4 + 1) * 512)
        c.op("dve", "tensor_tensor", [pb, bb], [ob], out=ot[:, sl], in0=pt[:], in1=bt[:], op=ALU.add)
    ot, ob = outs["geff"]
    c.op("dve", "scalar_tensor_tensor", [ob, gbcb], [ob], out=ot[:], in0=ot[:], scalar=1.0, in1=gbc[:],
         op0=ALU.add, op1=ALU.mult)


def norm_rows(c, xt, xb, outs, tmp, hb_t, hb_b):
    sq, sqb = tmp["sq"]
    ss, ssb = tmp["ss"]
    rs, rsb = tmp["rs"]
    h1, h1b = tmp["h1"]
    c.op("act", "activation", [xb], [sqb, ssb], out=sq[:], in_=xt, func=AF.Square, accum_out=ss[:])
    c.op("dve", "tensor_scalar", [ssb], [rsb], out=rs[:], in0=ss[:], scalar1=1.0 / D, scalar2=EPS,
         op0=ALU.mult, op1=ALU.add)
    c.op("act", "activation", [rsb], [rsb], out=rs[:], in_=rs[:], func=AF.Sqrt)
    c.op("dve", "reciprocal", [rsb], [rsb], out=rs[:], in_=rs[:])
    gt, gb = outs["geff"]
    st, sb = outs["shift"]
    c.op("dve", "scalar_tensor_tensor", [xb, rsb, gb], [h1b], out=h1[:], in0=xt, scalar=rs[:, 0:1], in1=gt[:],
         op0=ALU.mult, op1=ALU.mult)
    c.op("pool", "tensor_tensor", [h1b, sb], [hb_b], out=hb_t, in0=h1[:], in1=st[:], op=ALU.add)


def build_pool(stop=None):
    nc = bass.Bass("TRN2", target_bir_lowering=False)
    dt = nc.dram_tensor
    x_d = dt("xs", [NB, 128, D], F32, kind="ExternalInput").ap()
    xh_d = dt("xh", [128, D], F32, kind="ExternalInput").ap()
    tok_d = dt("tokrow", [1, NCOL], F32, kind="ExternalInput").ap()
    ccol_d = dt("ccol", [128, 16], F32, kind="ExternalInput").ap()
    modw_d = dt("modw", [D, 3 * D], F32, kind="ExternalInput").ap()
    modb_d = dt("modb", [1, 3 * D], F32, kind="ExternalInput").ap()
    g_d = dt("g", [1, D], F32, kind="ExternalInput").ap()
    win_d = dt("win", [D, 2 * D], F32, kind="ExternalInput").ap()
    wgrp_d = dt("wgrp", [4, 512, 512], F32, kind="ExternalInput").ap()
    ls_d = dt("lscol", [128, 16], F32, kind="ExternalInput").ap()
    wout_d = dt("wout", [D, D], F32, kind="ExternalInput").ap()
    xo_d = dt("xo", [NB, 128, D], F32, kind="ExternalOutput").ap()
    with ExitStack() as es:
        c = Ctx(nc, es)
        K = make_consts(c, nc)
        idt, idb = K["ident"]
        ps = [c.psum(f"ps{i}", [128, 512], F32) for i in range(8)]
        pTs = [(ps[6][0][:].bitcast(BF16), ps[6][1]), (ps[7][0][:].bitcast(BF16), ps[7][1])]
        outs = {n: c.sbuf(n + "_bc", [128, D], F32) for n in ("shift", "geff", "gate")}
        wsl = [c.sbuf(f"wsl{i}", [128, 16 * 512], BF16) for i in range(3)]
        xin = [c.sbuf(f"xin{i}", [128, D], F32) for i in range(2)]
        h1p = c.sbuf("h1", [128, D], F32)
        gbcp = c.sbuf("gbc", [128, D], F32)
        xo = [c.sbuf(f"xo{i}", [128, 512], F32) for i in range(2)]
        xr = [c.sbuf(f"xr{i}", [128, 512], F32) for i in range(2)]
        mod_step(c, nc, ccol_d, modw_d, modb_d, g_d, ps, outs, gbcp, xr, wsl)
        if stop == "mod":
            dbg_d = dt("dbg", [3, 128, D], F32, kind="ExternalOutput").ap()
            dbb = c.buf("dbg"); c.outbufs.append(dbb)
            for i, n in enumerate(("shift", "geff", "gate")):
                c.dma("sp", dbg_d[i], outs[n][0][:], [outs[n][1]], dbb)
            c.emit()
            return nc

        hT, hTb = c.sbuf("hT", [128, 16, NCOL], BF16)
        hbs = [c.sbuf(f"hb{i}", [128, D], BF16) for i in range(2)]
        tmp = {"sq": c.sbuf("sq", [128, D], BF16), "ss": c.sbuf("ss", [128, 1], F32),
               "rs": c.sbuf("rs", [128, 1], F32), "h1": h1p}
        for rb in range(NB + 1):
            xt, xb = xin[rb % 2]
            c.dma("sp", xt[:], x_d[rb] if rb < NB else xh_d, [], xb)
            ht, hb = hbs[rb % 2]
            norm_rows(c, xt[:], xb, outs, tmp, ht[:], hb)
            for q in range(4):
                half = q % 2
                pT, pb = pTs[half]
                for f in range(4):
                    fc = q * 4 + f
                    c.op("pe", "transpose", [hb, idb], [pb], pT[:, f * 128:(f + 1) * 128],
                         ht[:, fc * 128:(fc + 1) * 128], idt[:], signal=(f == 3))
                src = pT[:, 0:512]
                if rb < NB:
                    dst = hT[:, q * 4:(q + 1) * 4, rb * HC + 16: rb * HC + 144]
                    srcv = src.rearrange("p (f t) -> p f t", f=4)
                else:
                    dst = hT[:, q * 4:(q + 1) * 4, :].rearrange("p f (b c) -> p f b c", c=HC)[:, :, :, 0:16]
                    srcv = src.rearrange("p (f b r) -> p f b r", f=4, b=NB)
                eng = "act" if q % 2 == 0 else "dve"
                if eng == "act":
                    c.op("act", "activation", [pb], [hTb], out=dst, in_=srcv, func=AF.Copy)
                else:
                    c.op("dve", "tensor_copy", [pb], [hTb], out=dst, in_=srcv)

        if stop == "N":
            dbg_d = dt("dbg", [128, 16 * NCOL], BF16, kind="ExternalOutput").ap()
            dbb = c.buf("dbg"); c.outbufs.append(dbb)
            c.dma("sp", dbg_d, hT[:].rearrange("p k c -> p (k c)"), [hTb], dbb)
            c.emit()
            return nc
        tokbc, tokb = c.sbuf("tokbc", [128, NCOL], F32)
        c.dma("sp", tokbc[:], tok_d.partition_broadcast(128), [], tokb)
        vmask, vmb = c.sbuf("vmask", [128, NB, 16], F32)
        tok3 = tokbc[:].rearrange("p (b c) -> p b c", c=HC)
        c.op("dve", "tensor_scalar", [tokb], [vmb], out=vmask[:], in0=tok3[:, :, 0:16], scalar1=0.0, scalar2=None,
             op0=ALU.is_ge)
        cnt, cntb = c.sbuf("cnt", [128, NB, 128], F32)
        lscol, lsb = c.sbuf("lscol", [128, 16], F32)
        c.dma("sp", lscol[:], ls_d, [], lsb)
        wgs = [c.sbuf(f"wgs{i}", [128, 4, 512], BF16) for i in range(2)]
        def v3(pair):
            return (pair[0][:, 0:NCOL].rearrange("p (b c) -> p b c", c=HC), pair[1])
        vs_ = [v3(xin[0]), v3(xin[1])]
        lv = [v3(h1p), v3(gbcp)]
        pooled, pooledb = c.sbuf("pooled", [128, 4, NB * 128], BF16)
        sg = [c.sbuf(f"sg{i}", [128, 512], F32) for i in range(2)]
        uT = [(outs[n][0][:].bitcast(BF16).rearrange("p (k t) -> p k t", k=4), outs[n][1]) for n in ("shift", "geff")]
        xreg = {(b, mq): c.buf(f"xreg{b}_{mq}") for b in range(NB) for mq in range(2)}
        c.outbufs += list(xreg.values())
        winv = win_d.rearrange("(kc p) n -> p kc n", p=128)
        woutv = wout_d.rearrange("(cc p) m -> p cc m", p=128)
        wslot_i = 0
        xcount = 0
        for gi in range(4):
            w = WINS[gi]
            c.op("dve", "tensor_scalar", [tokb], [cntb], out=cnt[:], in0=tok3[:, :, 16:144], scalar1=1.0,
                 scalar2=float(w), op0=ALU.add, op1=ALU.min)
            c.op("dve", "reciprocal", [cntb], [cntb], out=cnt[:], in_=cnt[:])
            Wv_t, Wv_b = wsl[wslot_i % 3]; wslot_i += 1
            Wv = Wv_t[:].rearrange("p (k n) -> p k n", k=16)
            c.dma("pool", Wv, winv[:, :, gi * 512:(gi + 1) * 512], [], Wv_b)
            Wg_t, Wg_b = wsl[wslot_i % 3]; wslot_i += 1
            Wg = Wg_t[:].rearrange("p (k n) -> p k n", k=16)
            c.dma("pool", Wg, winv[:, :, D + gi * 512: D + (gi + 1) * 512], [], Wg_b)
            wgt, wgb = wgs[gi % 2]
            c.dma("pool", wgt[:], wgrp_d[gi].rearrange("(cc p) n -> p cc n", p=128), [], wgb)
            for fl in range(4):
                for kc in range(16):
                    for j in range(3):
                        pt, pb = ps[j]
                        c.op("pe", "matmul", [Wv_b, hTb], [pb], pt[:, 0:384], Wv[:, kc, fl * 128:(fl + 1) * 128],
                             hT[:, kc, j * 384:(j + 1) * 384], start=(kc == 0), stop=(kc == 15),
                             signal=(kc == 15))
                vt, vb = vs_[fl % 2]
                vflat = xin[fl % 2][0][:, 0:NCOL]
                for j in range(3):
                    pt, pb = ps[j]
                    c.op("act", "activation", [pb], [vb], out=vflat[:, j * 384:(j + 1) * 384], in_=pt[:, 0:384],
                         func=AF.Copy)
                c.op("dve", "tensor_tensor", [vb, vmb], [vb], out=vt[:, :, 0:16], in0=vt[:, :, 0:16], in1=vmask[:],
                     op=ALU.mult)
                cur, curb = vt, vb
                off = 0
                step = 1
                li = 0
                while step < w:
                    off += step
                    nt, nb_ = lv[li % 2]
                    li += 1
                    c.op("dve", "tensor_tensor", [curb], [nb_], out=nt[:, :, off:HC], in0=cur[:, :, off:HC],
                         in1=cur[:, :, off - step:HC - step], op=ALU.add)
                    cur, curb = nt, nb_
                    step *= 2
                nt, nb_ = lv[li % 2]
                c.op("dve", "tensor_tensor", [curb, cntb], [nb_], out=nt[:, :, 16:HC], in0=cur[:, :, 16:HC],
                     in1=cnt[:], op=ALU.mult)
                c.op("pool", "tensor_tensor", [nb_, vb], [pooledb],
                     out=pooled[:, fl, :].rearrange("p (b t) -> p b t", b=NB), in0=nt[:, :, 16:HC],
                     in1=vt[:, :, 16:HC], op=ALU.subtract)
            ut, ub = uT[gi % 2]
            for nl in range(4):
                nchunk = gi * 4 + nl
                for j in range(2):
                    pm, pmb = ps[3 + j]
                    for cc in range(4):
                        c.op("pe", "matmul", [wgb, pooledb], [pmb], pm[:], wgt[:, cc, nl * 128:(nl + 1) * 128],
                             pooled[:, cc, j * 512:(j + 1) * 512], start=(cc == 0), stop=(cc == 3),
                             signal=(cc == 3))
                    pg, pgb = ps[5 + j]
                    for kc in range(16):
                        rhs = hT[:, kc, :].rearrange("p (b c) -> p b c", c=HC)[:, j * 4:(j + 1) * 4, 16:HC]
                        c.op("pe", "matmul", [Wg_b, hTb], [pgb], pg[:], Wg[:, kc, nl * 128:(nl + 1) * 128], rhs,
                             start=(kc == 0), stop=(kc == 15), signal=(kc == 15))
                    st_, sb_ = sg[j]
                    c.op("act", "activation", [pgb], [sb_], out=st_[:], in_=pg[:], func=AF.Silu)
                    c.op("dve", "scalar_tensor_tensor", [pmb, lsb, sb_], [ub], out=ut[:, nl, j * 512:(j + 1) * 512],
                         in0=pm[:], scalar=lscol[:, nchunk:nchunk + 1], in1=st_[:], op0=ALU.mult, op1=ALU.mult)
            gt_, gb_ = outs["gate"]
            for mh in range(2):
                Wo_t, Wo_b = wsl[wslot_i % 3]; wslot_i += 1
                Wo = Wo_t[:, 0:4096].rearrange("p (k m) -> p k m", k=4)
                c.dma("pool", Wo, woutv[:, gi * 4:(gi + 1) * 4, mh * 1024:(mh + 1) * 1024], [], Wo_b)
                for b in range(NB):
                    for mq in range(2):
                        mcol = mh * 1024 + mq * 512
                        po, pob = ps[(b * 2 + mq) % 3]
                        for cc in range(4):
                            c.op("pe", "matmul", [ub, Wo_b], [pob], po[:], ut[:, cc, b * 128:(b + 1) * 128],
                                 Wo[:, cc, mq * 512:(mq + 1) * 512], start=(cc == 0), stop=(cc == 3),
                                 signal=(cc == 3))
                        xrt, xrb = xr[xcount % 2]
                        xot, xob = xo[xcount % 2]
                        xcount += 1
                        src = x_d if gi == 0 else xo_d
                        c.dma("sp", xrt[:], src[b, :, mcol:mcol + 512], [xreg[(b, mq)]] if gi > 0 else [], xrb)
                        c.op("dve", "tensor_tensor", [pob, gb_], [xob], out=xot[:], in0=po[:],
                             in1=gt_[:, mcol:mcol + 512], op=ALU.mult)
                        c.op("pool", "tensor_tensor", [xob, xrb], [xob], out=xot[:], in0=xot[:], in1=xrt[:],
                             op=ALU.add)
                        c.dma("sp", xo_d[b, :, mcol:mcol + 512], xot[:], [xob], xreg[(b, mq)])
        c.emit()
    return nc


ATT_IN = 9296
NH = 16
TWO_PI = 6.283185307179586


def rope_tables(c, pos_d, invf_d):
    posi, posib = c.sbuf("posi", [128, NB], I32)
    posf, posfb = c.sbuf("posf", [128, NB], F32)
    invf, invfb = c.sbuf("invf", [128, 24], F32)
    cs, csb = c.sbuf("cs", [128, NB, 2, 24], F32)
    yk, ykb = c.sbuf("yk", [128, NB, 2, 24], I32)
    yf, yfb = c.sbuf("yf", [128, NB, 2, 24], F32)
    m1, m1b = c.sbuf("rm1", [128, NB, 2, 24], F32)
    c.dma("sp", posi[:], pos_d, [], posib)
    c.dma("sp", invf[:], invf_d.partition_broadcast(128), [], invfb)
    c.op("dve", "tensor_copy", [posib], [posfb], out=posf[:], in_=posi[:])
    for j in range(NB):
        c.op("dve", "tensor_scalar", [posfb, invfb], [csb], out=cs[:, j, 1, :], in0=invf[:], scalar1=posf[:, j:j + 1],
             scalar2=1.0 / TWO_PI, op0=ALU.mult, op1=ALU.mult)
    c.op("dve", "tensor_scalar", [csb], [csb], out=cs[:, :, 0, :], in0=cs[:, :, 1, :], scalar1=0.25, scalar2=None,
         op0=ALU.add)
    c.op("dve", "tensor_copy", [csb], [ykb], out=yk[:], in_=cs[:])
    c.op("dve", "tensor_copy", [ykb], [yfb], out=yf[:], in_=yk[:])
    c.op("dve", "tensor_tensor", [csb, yfb], [csb], out=cs[:], in0=cs[:], in1=yf[:], op=ALU.subtract)
    c.op("dve", "tensor_scalar", [csb], [m1b], out=m1[:], in0=cs[:], scalar1=0.5, scalar2=None, op0=ALU.is_gt)
    c.op("dve", "tensor_tensor", [csb, m1b], [csb], out=cs[:], in0=cs[:], in1=m1[:], op=ALU.subtract)
    c.op("dve", "tensor_scalar", [csb], [m1b], out=m1[:], in0=cs[:], scalar1=-0.5, scalar2=None, op0=ALU.is_lt)
    c.op("dve", "tensor_tensor", [csb, m1b], [csb], out=cs[:], in0=cs[:], in1=m1[:], op=ALU.add)
    c.op("act", "activation", [csb], [csb], out=cs[:], in_=cs[:], func=AF.Sin, scale=TWO_PI)
    return cs, csb


def build_a1(stop=None):
    nc = bass.Bass("TRN2", target_bir_lowering=False)
    dt = nc.dram_tensor
    x_d = dt("xs", [NB, 128, D], F32, kind="ExternalInput").ap()
    ccol_d = dt("ccol", [128, 16], F32, kind="ExternalInput").ap()
    modw_d = dt("modw", [D, 3 * D], F32, kind="ExternalInput").ap()
    modb_d = dt("modb", [1, 3 * D], F32, kind="ExternalInput").ap()
    g_d = dt("g", [1, D], F32, kind="ExternalInput").ap()
    win_d = dt("win", [D, ATT_IN], F32, kind="ExternalInput").ap()
    pos_d = dt("posc", [128, NB], I32, kind="ExternalInput").ap()
    invf_d = dt("invf", [1, 24], F32, kind="ExternalInput").ap()
    QT_d = dt("QT", [NH, 128, NB * 128], BF16, kind="ExternalOutput").ap()
    KT_d = dt("KT", [NH, 128, NB * 128], BF16, kind="ExternalOutput").ap()
    SG_d = dt("SG", [NH, 128, NB * 128], BF16, kind="ExternalOutput").ap()
    V_d = dt("V", [NB * 128, D], BF16, kind="ExternalOutput").ap()
    QI_d = dt("QIT", [8, 128, NB * 128], BF16, kind="ExternalOutput").ap()
    KI_d = dt("KIT", [64, NB * 128], BF16, kind="ExternalOutput").ap()
    WI_d = dt("WI", [128, NB, 16], F32, kind="ExternalOutput").ap()
    GB_d = dt("GBC", [128, D], F32, kind="ExternalOutput").ap()
    with ExitStack() as es:
        c = Ctx(nc, es)
        K = make_consts(c, nc)
        idt, idb = K["ident"]
        ps = [c.psum(f"ps{i}", [128, 512], F32) for i in range(8)]
        pTs = [(ps[6][0][:].bitcast(BF16), ps[6][1]), (ps[7][0][:].bitcast(BF16), ps[7][1])]
        outs = {n: c.sbuf(n + "_bc", [128, D], F32) for n in ("shift", "geff", "gate")}
        wsl = [c.sbuf(f"wsl{i}", [128, 16 * 512], BF16) for i in range(3)]
        xin = [c.sbuf(f"xin{i}", [128, D], F32) for i in range(2)]
        h1p = c.sbuf("h1", [128, D], F32)
        gbcp = c.sbuf("gbc", [128, D], F32)
        xr = [c.sbuf(f"xr{i}", [128, 512], F32) for i in range(2)]
        mod_step(c, nc, ccol_d, modw_d, modb_d, g_d, ps, outs, gbcp, xr, wsl)
        ob = {n: c.buf("o_" + n) for n in ("QT", "KT", "SG", "V", "QI", "KI", "WI", "GB")}
        c.outbufs += list(ob.values())
        c.dma("sp", GB_d, outs["gate"][0][:], [outs["gate"][1]], ob["GB"])
        cs, csb = rope_tables(c, pos_d, invf_d)
        if stop == "rope":
            dbg_d = dt("dbg", [128, NB * 48], F32, kind="ExternalOutput").ap()
            c.dma("sp", dbg_d, cs[:].rearrange("p a b f -> p (a b f)"), [csb], c.buf("dbg"))
            c.emit()
            return nc
        hT, hTb = c.sbuf("hT", [128, 16, NB * 128], BF16)
        hbs = [c.sbuf(f"hb{i}", [128, D], BF16) for i in range(2)]
        tmp = {"sq": c.sbuf("sq", [128, D], BF16), "ss": c.sbuf("ss", [128, 1], F32),
               "rs": c.sbuf("rs", [128, 1], F32), "h1": h1p}
        for rb in range(NB):
            xt, xb = xin[rb % 2]
            c.dma("sp", xt[:], x_d[rb], [], xb)
            ht, hb = hbs[rb % 2]
            norm_rows(c, xt[:], xb, outs, tmp, ht[:], hb)
            for q in range(4):
                pT, pb = pTs[q % 2]
                for f in range(4):
                    fc = q * 4 + f
                    c.op("pe", "transpose", [hb, idb], [pb], pT[:, f * 128:(f + 1) * 128],
                         ht[:, fc * 128:(fc + 1) * 128], idt[:], signal=(f == 3))
                dst = hT[:, q * 4:(q + 1) * 4, rb * 128:(rb + 1) * 128]
                srcv = pT[:, 0:512].rearrange("p (f t) -> p f t", f=4)
                if q % 2 == 0:
                    c.op("act", "activation", [pb], [hTb], out=dst, in_=srcv, func=AF.Copy)
                else:
                    c.op("dve", "tensor_copy", [pb], [hTb], out=dst, in_=srcv)
        cexp = []
        for nm, nh_e, half_e, f0_e in (("cq", 4, 16, 0), ("ci", 8, 8, 16)):
            ce, ceb = c.sbuf(nm, [128, NB, 2, 64], F32)
            for b in range(NB):
                for t_ in range(2):
                    c.op("dve", "tensor_copy", [csb], [ceb], out=ce[:, b, t_, :].rearrange("p (h d) -> p h d", h=nh_e),
                         in_=cs[:, b, t_, f0_e:f0_e + half_e].unsqueeze(1).to_broadcast([128, nh_e, half_e]))
            cexp.append((ce, ceb))
        winv = win_d.rearrange("(kc p) n -> p kc n", p=128)
        tms = [c.sbuf(f"tm{i}", [128, 512], BF16) for i in range(2)]
        xfs = [c.sbuf(f"xf{i}", [128, 512], F32) for i in range(2)]
        stg = [c.sbuf(f"stg{i}", [128, 512], BF16) for i in range(2)]
        rt = [c.sbuf(f"rt{i}", [128, 64], F32) for i in range(4)]
        wif = [c.sbuf(f"wif{i}", [128, 16], F32) for i in range(2)]
        chunks = list(range(19)) if stop is None else stop
        it = 0
        for ch in chunks:
            c0 = ch * 512
            ncol = 512 if ch < 18 else ATT_IN - 18 * 512
            Wt, Wb = wsl[ch % 3]
            W = Wt[:, 0:16 * ncol].rearrange("p (k n) -> p k n", k=16)
            c.dma("pool", W, winv[:, :, c0:c0 + ncol], [], Wb)
            kind = ("q", "k", "v", "g", "qi")[ch // 4] if ch < 18 else "ki"
            for b in range(NB):
                pt, pb = ps[it % 4]
                tm, tmb = tms[it % 2]
                sg_, sgb = stg[it % 2]
                pT, pTb_ = pTs[it % 2]
                it += 1
                for kc in range(16):
                    c.op("pe", "matmul", [hTb, Wb], [pb], pt[:, 0:ncol], hT[:, kc, b * 128:(b + 1) * 128], W[:, kc, :],
                         start=(kc == 0), stop=(kc == 15), signal=(kc == 15))
                tsl = slice(b * 128, (b + 1) * 128)
                if kind == "v":
                    c.op("act", "activation", [pb], [tmb], out=tm[:], in_=pt[:], func=AF.Copy)
                    c.dma("sp", V_d[tsl, (ch - 8) * 512:(ch - 7) * 512], tm[:], [tmb], ob["V"], nowaw=True, semof=tmb)
                    continue
                if kind == "g":
                    c.op("act", "activation", [pb], [tmb], out=tm[:], in_=pt[:], func=AF.Silu)
                else:
                    if kind in ("q", "k"):
                        nh_, hd, half, f0 = 4, 128, 16, 0
                    elif kind == "qi":
                        nh_, hd, half, f0 = 8, 64, 8, 16
                    else:
                        nh_, hd, half, f0 = 1, 64, 8, 16
                    wcol = nh_ * hd
                    xf_, xfb_ = xfs[it % 2]
                    c.op("act", "activation", [pb], [xfb_], out=xf_[:, 0:wcol], in_=pt[:, 0:wcol], func=AF.Copy)
                    c.op("act", "activation", [pb], [tmb], out=tm[:, 0:wcol], in_=pt[:, 0:wcol], func=AF.Copy)
                    pv = xf_[:, 0:wcol].rearrange("p (h d) -> p h d", h=nh_)
                    tv = tm[:, 0:wcol].rearrange("p (h d) -> p h d", h=nh_)
                    rsb_ = xfb_
                    ceb_ = csb
                    if kind == "ki":
                        cosb = cs[:, b, 0, f0:f0 + half].unsqueeze(1)
                        sinb = cs[:, b, 1, f0:f0 + half].unsqueeze(1)
                    else:
                        ce, ceb_ = cexp[0] if kind in ("q", "k") else cexp[1]
                        cosb = ce[:, b, 0, :].rearrange("p (h d) -> p h d", h=nh_)
                        sinb = ce[:, b, 1, :].rearrange("p (h d) -> p h d", h=nh_)
                    x1 = pv[:, :, 0:half]
                    x2 = pv[:, :, half:2 * half]
                    tt = [(rt[i][0][:, 0:nh_ * half].rearrange("p (h d) -> p h d", h=nh_), rt[i][1]) for i in range(4)]
                    c.op("dve", "tensor_tensor", [rsb_, ceb_], [tt[0][1]], out=tt[0][0], in0=x1, in1=cosb, op=ALU.mult,
                         signal=False)
                    c.op("dve", "tensor_tensor", [rsb_, ceb_], [tt[1][1]], out=tt[1][0], in0=x2, in1=sinb, op=ALU.mult,
                         signal=False)
                    c.op("dve", "tensor_tensor", [rsb_, ceb_], [tt[2][1]], out=tt[2][0], in0=x2, in1=cosb, op=ALU.mult,
                         signal=False)
                    c.op("dve", "tensor_tensor", [rsb_, ceb_], [tt[3][1]], out=tt[3][0], in0=x1, in1=sinb, op=ALU.mult)
                    c.op("dve", "tensor_tensor", [tt[0][1], tt[1][1]], [tmb], out=tv[:, :, 0:half], in0=tt[0][0],
                         in1=tt[1][0], op=ALU.subtract, signal=False)
                    c.op("dve", "tensor_tensor", [tt[2][1], tt[3][1]], [tmb], out=tv[:, :, half:2 * half], in0=tt[2][0],
                         in1=tt[3][0], op=ALU.add)
                    if kind == "ki":
                        wt_, wb_ = wif[b % 2]
                        c.op("act", "activation", [pb], [wb_], out=wt_[:], in_=pt[:, 64:80], func=AF.Copy)
                        c.dma("sp", WI_d[:, b, :], wt_[:], [wb_], ob["WI"], nowaw=True, semof=wb_)
                if kind == "ki":
                    c.op("pe", "transpose", [tmb, idb], [pTb_], pT[:, 0:128], tm[:, 0:128], idt[:])
                    c.op("dve", "tensor_copy", [pTb_], [sgb], out=sg_[0:64, 0:128], in_=pT[0:64, 0:128])
                    c.dma("sp", KI_d[:, tsl], sg_[0:64, 0:128], [sgb], ob["KI"], nowaw=True, semof=sgb)
                    continue
                for f in range(4):
                    c.op("pe", "transpose", [tmb, idb], [pTb_], pT[:, f * 128:(f + 1) * 128],
                         tm[:, f * 128:(f + 1) * 128], idt[:], signal=(f == 3))
                c.op("dve", "tensor_copy", [pTb_], [sgb], out=sg_[:], in_=pT[:, 0:512])
                dd, key, hc = {"q": (QT_d, "QT", ch), "k": (KT_d, "KT", ch - 4), "g": (SG_d, "SG", ch - 12),
                               "qi": (QI_d, "QI", ch - 16)}[kind]
                dst = dd[hc * 4:(hc + 1) * 4, :, tsl].rearrange("h d t -> d h t")
                c.dma("sp", dst, sg_[:].rearrange("p (h t) -> p h t", h=4), [sgb], ob[key], nowaw=True, semof=sgb)
        c.emit()
    return nc


NIT = 20
TOPK = 256
BIG = 1.0e30


def mt_base(kb):
    J = kb // 8
    return sum(8 * (8 - jj) for jj in range(J)) + (kb - 8 * J) * (8 - J)


def build_a2(stop=None):
    nc = bass.Bass("TRN2", target_bir_lowering=False)
    dt = nc.dram_tensor
    S = NB * 128 * NCORE
    x_d = dt("xs", [NB, 128, D], F32, kind="ExternalInput").ap()
    GB_d = dt("GBC", [128, D], F32, kind="ExternalInput").ap()
    QT_d = dt("QT", [NH, 128, NB * 128], BF16, kind="ExternalInput").ap()
    SG_d = dt("SG", [NH, 128, NB * 128], BF16, kind="ExternalInput").ap()
    QI_d = dt("QIT", [8, 128, NB * 128], BF16, kind="ExternalInput").ap()
    WI_d = dt("WI", [128, NB, 16], F32, kind="ExternalInput").ap()
    KT_d = dt("KTg", [NH, 128, S], BF16, kind="ExternalInput").ap()
    V_d = dt("Vg", [NH, 128, 64, 128], BF16, kind="ExternalInput").ap()
    KI_d = dt("KITg", [64, S], BF16, kind="ExternalInput").ap()
    trel_d = dt("trel", [128, 1], F32, kind="ExternalInput").ap()
    wout_d = dt("wout", [D, D], F32, kind="ExternalInput").ap()
    xo_d = dt("xo", [NB, 128, D], F32, kind="ExternalOutput").ap()
    with ExitStack() as es:
        c = Ctx(nc, es)
        K = make_consts(c, nc)
        idt, idb = K["ident"]
        ps = [c.psum(f"ps{i}", [128, 512], F32) for i in range(8)]
        pTs = [(ps[6][0][:].bitcast(BF16), ps[6][1]), (ps[7][0][:].bitcast(BF16), ps[7][1])]
        mT, mTb = c.sbuf("mT", [128, 288, 128], BF16)
        ones, onesb = c.sbuf("ones", [128, 128], BF16)
        c.op("pool", "memset", [], [onesb], ones[:], 1.0)
        with ExitStack() as es2:
            c.es = es2
            ki2, ki2b = c.sbuf("ki2", [128, S], BF16)
            c.dma("sp", ki2[0:64, :], KI_d, [], ki2b)
            c.dma("sp", ki2[64:128, :], KI_d, [], ki2b)
            qit, qitb = c.sbuf("qit", [128, 8, NB * 128], BF16)
            c.dma("sp", qit[:], QI_d.rearrange("c p t -> p c t"), [], qitb)
            wi, wib = c.sbuf("wi", [128, NB, 16], F32)
            c.dma("sp", wi[:], WI_d, [], wib)
            trel, trelb = c.sbuf("trel", [128, 1], F32)
            c.dma("sp", trel[:], trel_d, [], trelb)
            ioi, ioib = c.sbuf("ioi", [128, 1024], I32)
            cm, cmb = c.sbuf("cm", [128, 1024], F32)
            nbias, nbb = c.sbuf("nbias", [128, 1024], F32)
            c.op("pool", "iota", [], [ioib], ioi[:], pattern=[[1, 1024]], base=0, channel_multiplier=0)
            c.op("dve", "tensor_copy", [ioib], [cmb], out=cm[:], in_=ioi[:])
            c.op("dve", "tensor_scalar", [cmb, trelb], [cmb], out=cm[:], in0=cm[:], scalar1=trel[:, 0:1], scalar2=None,
                 op0=ALU.is_le)
            c.op("dve", "tensor_scalar", [cmb], [nbb], out=nbias[:], in0=cm[:], scalar1=-1.0, scalar2=BIG,
                 op0=ALU.add, op1=ALU.mult)
            p2, p2b = c.sbuf("pow2", [128, NIT + 2], F32)
            for k in range(NIT + 2):
                c.op("pool", "memset", [], [p2b], p2[:, k:k + 1], float(2.0 ** (-k)))
            Sc, Scb = c.sbuf("Sc", [128, S], F32)
            mk, mkb = c.sbuf("mk", [128, S], BF16)
            rr = [c.sbuf(f"rr{i}", [128, 512], F32) for i in range(4)]
            accB, accBb = c.sbuf("accB", [128, 512], F32)
            sm = {n: c.sbuf("bs_" + n, [128, 1], F32) for n in ("mn", "mx", "W", "cand", "cnt", "u", "thr")}
            steps, stepsb = c.sbuf("steps", [128, NIT + 2], F32)
            it = 0
            for j in range(NB):
                n = 1024 * (j + 1)
                tsl = slice(j * 128, (j + 1) * 128)
                for kt in range(2 * (j + 1)):
                    ksl = slice(kt * 512, (kt + 1) * 512)
                    for h in range(16):
                        par = h % 2
                        psl = slice(par * 64, (par + 1) * 64)
                        pt, pb = ps[it % 6]
                        rt_, rb_ = rr[it % 4]
                        it += 1
                        c.op("pe", "matmul", [qitb, ki2b], [pb], pt[:], qit[psl, h // 2, tsl], ki2[psl, ksl],
                             start=True, stop=True)
                        c.op("act", "activation", [pb], [rb_], out=rt_[:], in_=pt[:], func=AF.Relu)
                        eng, at, ab = "dve", Sc[:, ksl], Scb
                        if h < 1:
                            c.op(eng, "tensor_scalar", [rb_, wib], [ab], out=at, in0=rt_[:], scalar1=wi[:, j, h:h + 1],
                                 scalar2=None, op0=ALU.mult)
                        elif eng == "dve":
                            c.op(eng, "scalar_tensor_tensor", [rb_, wib, ab], [ab], out=at, in0=rt_[:],
                                 scalar=wi[:, j, h:h + 1], in1=at, op0=ALU.mult, op1=ALU.add)
                        else:
                            c.op(eng, "tensor_scalar", [rb_, wib], [rb_], out=rt_[:], in0=rt_[:],
                                 scalar1=wi[:, j, h:h + 1], scalar2=None, op0=ALU.mult)
                            c.op(eng, "tensor_tensor", [rb_, ab], [ab], out=at, in0=at, in1=rt_[:], op=ALU.add)
                mn, mnb = sm["mn"]; mx, mxb = sm["mx"]; W_, Wb_ = sm["W"]; cand, candb = sm["cand"]
                cnt, cntb = sm["cnt"]; u_, ub_ = sm["u"]; thr, thrb = sm["thr"]
                c.op("dve", "tensor_reduce", [Scb], [mnb], out=mn[:], in_=Sc[:, 0:n], axis=AX.X, op=ALU.min)
                c.op("dve", "tensor_reduce", [Scb], [mxb], out=mx[:], in_=Sc[:, 0:n], axis=AX.X, op=ALU.max)
                c.op("dve", "tensor_tensor", [mxb, mnb], [Wb_], out=W_[:], in0=mx[:], in1=mn[:], op=ALU.subtract)
                c.op("dve", "tensor_scalar", [Wb_, p2b], [stepsb], out=steps[:], in0=p2[:], scalar1=W_[:, 0:1], scalar2=None,
                     op0=ALU.mult)
                c.op("dve", "tensor_tensor", [mnb, stepsb], [candb], out=cand[:], in0=mn[:], in1=steps[:, 1:2],
                     op=ALU.add)
                lsl = slice(n - 1024, n)
                c.op("dve", "tensor_tensor", [Scb, cmb], [Scb], out=Sc[:, lsl], in0=Sc[:, lsl], in1=cm[:], op=ALU.mult)
                c.op("dve", "tensor_tensor", [Scb, nbb], [Scb], out=Sc[:, lsl], in0=Sc[:, lsl], in1=nbias[:], op=ALU.add)
                for k in range(1, NIT + 1):
                    c.op("dve", "tensor_scalar", [Scb, candb], [mkb, cntb], out=mk[:, 0:n], in0=Sc[:, 0:n],
                         scalar1=cand[:, 0:1], scalar2=None, op0=ALU.is_ge, op1=ALU.add, accum_out=cnt[:])
                    c.op("dve", "tensor_scalar", [cntb], [ub_], out=u_[:], in0=cnt[:], scalar1=float(TOPK) - 0.5,
                         scalar2=0.5, op0=ALU.is_ge, op1=ALU.subtract)
                    c.op("dve", "scalar_tensor_tensor", [ub_, stepsb, candb], [candb], out=cand[:], in0=u_[:],
                         scalar=steps[:, k:k + 1], in1=cand[:], op0=ALU.mult, op1=ALU.add)
                c.op("dve", "scalar_tensor_tensor", [stepsb, candb], [thrb], out=thr[:], in0=steps[:, NIT + 1:NIT + 2],
                     scalar=-2.0, in1=cand[:], op0=ALU.mult, op1=ALU.add)
                c.op("dve", "tensor_scalar", [Scb, thrb], [mkb], out=mk[:, 0:n], in0=Sc[:, 0:n], scalar1=thr[:, 0:1],
                     scalar2=None, op0=ALU.is_ge)
                for g4 in range(2 * (j + 1)):
                    kb0 = g4 * 4
                    J = kb0 // 8
                    pT, pTb_ = pTs[g4 % 2]
                    for f in range(4):
                        kb = kb0 + f
                        c.op("pe", "transpose", [mkb, idb], [pTb_], pT[:, f * 128:(f + 1) * 128],
                             mk[:, kb * 128:(kb + 1) * 128], idt[:], signal=(f == 3))
                    b0 = mt_base(8 * J)
                    dst = mT[:, b0:b0 + 8 * (8 - J), :].rearrange("p (kb jj) t -> p kb jj t", jj=8 - J)[
                        :, kb0 - 8 * J:kb0 - 8 * J + 4, j - J, :]
                    src = pT[:, 0:512].rearrange("p (f t) -> p f t", f=4)
                    if g4 % 2 == 0:
                        c.op("act", "activation", [pTb_], [mTb], out=dst, in_=src, func=AF.Copy)
                    else:
                        c.op("pool", "tensor_copy", [pTb_], [mTb], out=dst, in_=src) if False else \
                            c.op("dve", "tensor_copy", [pTb_], [mTb], out=dst, in_=src)
            c.barrier()
        c.es = es
        kts = [c.sbuf(f"kts{i}", [128, S], BF16) for i in range(1)]
        vts = [c.sbuf(f"vts{i}", [128, 64, 128], BF16) for i in range(1)]
        qts = [c.sbuf(f"qts{i}", [128, NB * 128], BF16) for i in range(2)]
        sgs = [c.sbuf(f"sgs{i}", [128, NB * 128], BF16) for i in range(2)]
        pex = [c.sbuf(f"pex{i}", [128, 512], BF16) for i in range(3)]
        pms = [c.sbuf(f"pms{i}", [128, 512], BF16) for i in range(3)]
        ogT, ogTb = c.sbuf("ogT", [128, NH, NB * 128], BF16)
        rl, rlb = c.sbuf("rl", [128, 512], F32)
        on_, onb = c.sbuf("on", [128, 512], F32)
        scale = float(128 ** -0.5)
        it = 0
        hh = 0
        for h in range(NH):
            ktt, ktb = kts[0]
            vtt, vtb = vts[0]
            qtt, qtb = qts[h % 2]
            sgt, sgb = sgs[h % 2]
            for q4 in range(4):
                c.dma("sp", ktt[:, q4 * 2048:(q4 + 1) * 2048], KT_d[h, :, q4 * 2048:(q4 + 1) * 2048], [], ktb,
                      nowaw=(q4 > 0))
                c.dma("sp", vtt[:, q4 * 16:(q4 + 1) * 16, :], V_d[h, :, q4 * 16:(q4 + 1) * 16, :], [],
                      vtb, nowaw=(q4 > 0))
            c.dma("sp", qtt[:], QT_d[h], [], qtb)
            c.dma("sp", sgt[:], SG_d[h], [], sgb)
            for half in range(2):
                jlo = 4 * half
                po, pob = ps[4 + 2 * (hh % 2)]
                pl, plb = ps[5 + 2 * (hh % 2)]
                hh += 1
                nkb = 8 * (jlo + 4)
                for kb in range(nkb):
                    J = kb // 8
                    j0 = max(J, jlo)
                    nj = jlo + 4 - j0
                    N = 128 * nj
                    t0 = 128 * j0
                    pL, pLb = ps[it % 4]
                    pe_, peb = pex[it % 3]
                    pm_, pmb = pms[it % 3]
                    it += 1
                    c.op("pe", "matmul", [ktb, qtb], [pLb], pL[:, 0:N], ktt[:, kb * 128:(kb + 1) * 128], qtt[:, t0:t0 + N],
                         start=True, stop=True)
                    c.op("act", "activation", [pLb], [peb], out=pe_[:, 0:N], in_=pL[:, 0:N], func=AF.Exp, scale=scale)
                    mb0 = mt_base(kb) + (j0 - J)
                    eng = "dve"
                    c.op(eng, "tensor_tensor", [peb, mTb], [pmb], out=pm_[:, 0:N], in0=pe_[:, 0:N],
                         in1=mT[:, mb0:mb0 + nj, :].rearrange("p a t -> p (a t)"), op=ALU.mult)
                    osl = slice((j0 - jlo) * 128, 512)
                    st_, sp_ = (kb == 0), (kb == nkb - 1)
                    c.op("pe", "matmul", [vtb, pmb], [pob], po[:, osl], vtt[:, kb, :], pm_[:, 0:N],
                         start=st_, stop=sp_, signal=False)
                    c.op("pe", "matmul", [onesb, pmb], [plb], pl[:, osl], ones[:], pm_[:, 0:N],
                         start=st_, stop=sp_, signal=True)
                hsl = slice(half * 512, (half + 1) * 512)
                c.op("dve", "reciprocal", [plb], [rlb], out=rl[:], in_=pl[:])
                c.op("dve", "tensor_tensor", [pob, rlb], [onb], out=on_[:], in0=po[:], in1=rl[:], op=ALU.mult)
                c.op("pool", "tensor_tensor", [onb, sgb], [ogTb], out=ogT[:, h, hsl], in0=on_[:], in1=sgt[:, hsl],
                     op=ALU.mult)
        gbc, gbcb = c.sbuf("gbc", [128, D], F32)
        c.dma("sp", gbc[:], GB_d, [], gbcb)
        wsl = [c.sbuf(f"wsl{i}", [128, 16, 512], BF16) for i in range(2)]
        xo = [c.sbuf(f"xo{i}", [128, 512], F32) for i in range(2)]
        xr = [c.sbuf(f"xr{i}", [128, 512], F32) for i in range(2)]
        xod = c.buf("xo_d")
        woutv = wout_d.rearrange("(kc p) m -> p kc m", p=128)
        it = 0
        for mc in range(4):
            Wt, Wb = wsl[mc % 2]
            msl = slice(mc * 512, (mc + 1) * 512)
            c.dma("pool", Wt[:], woutv[:, :, msl], [], Wb)
            for b in range(NB):
                po, pob = ps[it % 4]
                xrt, xrb = xr[it % 2]
                xot, xob = xo[it % 2]
                it += 1
                for hc in range(16):
                    c.op("pe", "matmul", [ogTb, Wb], [pob], po[:], ogT[:, hc, b * 128:(b + 1) * 128], Wt[:, hc, :],
                         start=(hc == 0), stop=(hc == 15), signal=(hc == 15))
                c.dma("sp", xrt[:], x_d[b, :, msl], [], xrb)
                c.op("dve", "tensor_tensor", [pob, gbcb], [xob], out=xot[:], in0=po[:], in1=gbc[:, msl], op=ALU.mult)
                c.op("pool", "tensor_tensor", [xob, xrb], [xob], out=xot[:], in0=xot[:], in1=xrt[:], op=ALU.add)
                c.dma("sp", xo_d[b, :, msl], xot[:], [xob], xod, nowaw=True, semof=xob)
        c.emit()
    return nc


def build_final():
    nc = bass.Bass("TRN2", target_bir_lowering=False)
    dt = nc.dram_tensor
    x_d = dt("xs", [NB, 128, D], F32, kind="ExternalInput").ap()
    g_d = dt("g", [1, D], F32, kind="ExternalInput").ap()
    xo_d = dt("xo", [NB, 128, D], F32, kind="ExternalOutput").ap()
    with ExitStack() as es:
        c = Ctx(nc, es)
        gbc, gbcb = c.sbuf("gbc", [128, D], F32)
        c.dma("sp", gbc[:], g_d.partition_broadcast(128), [], gbcb)
        xin = [c.sbuf(f"xin{i}", [128, D], F32) for i in range(2)]
        xo = [c.sbuf(f"xo{i}", [128, D], F32) for i in range(2)]
        sq, sqb = c.sbuf("sq", [128, D], BF16)
        ss, ssb = c.sbuf("ss", [128, 1], F32)
        rs, rsb = c.sbuf("rs", [128, 1], F32)
        xod = c.buf("xo_d")
        for b in range(NB):
            xt, xb = xin[b % 2]
            ot, ob = xo[b % 2]
            c.dma("sp", xt[:], x_d[b], [], xb)
            c.op("act", "activation", [xb], [sqb, ssb], out=sq[:], in_=xt[:], func=AF.Square, accum_out=ss[:])
            c.op("dve", "tensor_scalar", [ssb], [rsb], out=rs[:], in0=ss[:], scalar1=1.0 / D, scalar2=EPS,
                 op0=ALU.mult, op1=ALU.add)
            c.op("act", "activation", [rsb], [rsb], out=rs[:], in_=rs[:], func=AF.Sqrt)
            c.op("dve", "reciprocal", [rsb], [rsb], out=rs[:], in_=rs[:])
            c.op("dve", "scalar_tensor_tensor", [xb, rsb, gbcb], [ob], out=ot[:], in0=xt[:], scalar=rs[:, 0:1],
                 in1=gbc[:], op0=ALU.mult, op1=ALU.mult)
            c.dma("sp", xo_d[b], ot[:], [ob], xod, nowaw=True, semof=ob)
        c.emit()
    return nc


def _gb(i, j):
    return 8 * j + i


def _shard_x(x, i):
    return np.ascontiguousarray(np.stack([x[128 * _gb(i, j):128 * _gb(i, j) + 128] for j in range(NB)]))


def _unshard_x(shards):
    out = np.zeros((NB * NCORE * 128, D), np.float32)
    for i in range(NCORE):
        for j in range(NB):
            out[128 * _gb(i, j):128 * _gb(i, j) + 128] = shards[i][j]
    return out


def _halo(x, i):
    xh = np.zeros((128, D), np.float32)
    tok = np.zeros((1, NCOL), np.float32)
    for j in range(NB):
        s = 128 * _gb(i, j)
        if s >= 16:
            xh[j * 16:(j + 1) * 16] = x[s - 16:s]
        tok[0, j * HC:(j + 1) * HC] = np.arange(s - 16, s + 128)
    return xh, tok


def _invf_row():
    a = 500000.0 ** (-np.arange(16, dtype=np.float32) * 2.0 / 32)
    b = 500000.0 ** (-np.arange(8, dtype=np.float32) * 2.0 / 16)
    return np.concatenate([a, b]).astype(np.float32)[None]


def _run(nc, maps):
    return run_bass_kernel_spmd(nc, maps, core_ids=list(range(NCORE))).results


def kernel(x, c, positions, norm_g, mod_w, mod_b, pool_w_in, pool_w_grp, pool_scale, pool_w_out,
           attn_w_in, attn_w_out, final_g):
    f32 = np.float32
    xc = np.ascontiguousarray(np.asarray(x, f32)[0])
    ccol = np.ascontiguousarray(np.asarray(c, f32)[0].reshape(16, 128).T)
    pos = np.asarray(positions)[0].astype(np.int32)
    ncs = {}

    def prog(name, fn):
        if name not in ncs:
            ncs[name] = fn()
        return ncs[name]

    for li in range(4):
        j2 = li // 2
        common = dict(ccol=ccol, modw=np.ascontiguousarray(mod_w[li], f32), modb=np.asarray(mod_b[li], f32)[None],
                      g=np.asarray(norm_g[li], f32)[None])
        if li % 2 == 0:
            maps = []
            lscol = np.ascontiguousarray(np.asarray(pool_scale[j2], f32).reshape(16, 128).T)
            for i in range(NCORE):
                xh, tok = _halo(xc, i)
                maps.append(dict(xs=_shard_x(xc, i), xh=xh, tokrow=tok, win=np.asarray(pool_w_in[j2], f32),
                                 wgrp=np.asarray(pool_w_grp[j2], f32), lscol=lscol,
                                 wout=np.asarray(pool_w_out[j2], f32), **common))
            res = _run(prog("pool", build_pool), maps)
            xc = _unshard_x([r["xo"] for r in res])
        else:
            maps = []
            for i in range(NCORE):
                posc = np.ascontiguousarray(
                    np.stack([pos[128 * _gb(i, j):128 * _gb(i, j) + 128] for j in range(NB)], axis=1))
                maps.append(dict(xs=_shard_x(xc, i), win=np.asarray(attn_w_in[j2], f32), posc=posc,
                                 invf=_invf_row(), **common))
            r1 = _run(prog("a1", build_a1), maps)
            S = NB * NCORE * 128
            bf = ml_dtypes.bfloat16
            KTg = np.zeros((NH, 128, S), bf)
            Vg = np.zeros((S, D), bf)
            KITg = np.zeros((64, S), bf)
            for i in range(NCORE):
                for j in range(NB):
                    gs = slice(128 * _gb(i, j), 128 * _gb(i, j) + 128)
                    ls = slice(128 * j, 128 * j + 128)
                    KTg[:, :, gs] = r1[i]["KT"][:, :, ls]
                    Vg[gs] = r1[i]["V"][ls]
                    KITg[:, gs] = r1[i]["KIT"][:, ls]
            Vg2 = np.ascontiguousarray(Vg.reshape(64, 128, NH, 128).transpose(2, 1, 0, 3))
            maps = []
            for i in range(NCORE):
                maps.append(dict(xs=_shard_x(xc, i), GBC=r1[i]["GBC"], QT=r1[i]["QT"], SG=r1[i]["SG"], QIT=r1[i]["QIT"],
                                 WI=r1[i]["WI"], KTg=KTg, Vg=Vg2, KITg=KITg,
                                 trel=(128 * i + np.arange(128, dtype=f32))[:, None],
                                 wout=np.asarray(attn_w_out[j2], f32)))
            r2 = _run(prog("a2", build_a2), maps)
            xc = _unshard_x([r["xo"] for r in r2])
    maps = [dict(xs=_shard_x(xc, i), g=np.asarray(final_g, f32)[None]) for i in range(NCORE)]
    rf = _run(prog("final", build_final), maps)
    out = _unshard_x([r["xo"] for r in rf])
    return out[None].astype(np.float32)
```

```python
import numpy as np
import ml_dtypes
from contextlib import ExitStack
import concourse.bass as bass
import concourse.mybir as mybir
from concourse.bass_utils import run_bass_kernel_spmd

F32 = mybir.dt.float32
BF16 = mybir.dt.bfloat16
I32 = mybir.dt.int32
ALU = mybir.AluOpType
AF = mybir.ActivationFunctionType
AX = mybir.AxisListType


class Buf:
    __slots__ = ("name", "lw", "rd", "dsem", "dval")

    def __init__(self, name):
        self.name = name
        self.lw = None
        self.rd = []
        self.dsem = None
        self.dval = 0


class Eng:
    def __init__(self, name):
        self.name = name
        self.sem = None
        self.count = 0
        self.ops = []
        self.waited = {}


class Ctx:
    def __init__(self, nc, es):
        self.nc = nc
        self.es = es
        self.es0 = es
        self.eng = {n: Eng(n) for n in ("pe", "act", "dve", "pool", "sp")}
        for n in ("pe", "act", "dve", "pool"):
            self.eng[n].sem = es.enter_context(nc.semaphore("s_" + n))
        self.nsem = 4
        self.outbufs = []
        self.dmabufs = []

    def buf(self, name):
        return Buf(name)

    def sbuf(self, name, shape, dt):
        t = self.es.enter_context(self.nc.sbuf_tensor("sb_" + name, list(shape), dt))
        return t, Buf(name)

    def psum(self, name, shape, dt):
        t = self.es.enter_context(self.nc.psum_tensor("pp_" + name, list(shape), dt))
        return t, Buf(name)

    def _wait(self, C, dep):
        if dep[0] == "eng":
            E, idx = dep[1], dep[2]
            assert E.count >= idx, f"dep on unsignaled instr {E.name} {idx} {E.count}"
            sem, val = E.sem, idx
        else:
            sem, val = dep[1], dep[2]
        k = id(sem)
        if C.waited.get(k, 0) >= val:
            return
        C.waited[k] = val
        C.ops.append(("wait", sem, val))

    def _deps(self, C, reads, writes, is_dma):
        deps = []
        for r in reads:
            if r.lw is not None:
                d = r.lw
                if d[0] == "eng" and d[1] is C and C.name == "pe":
                    continue
                deps.append(d)
        for w in writes:
            if w.lw is not None:
                d = w.lw
                if not (d[0] == "eng" and d[1] is C and not is_dma):
                    deps.append(d)
            for d in w.rd:
                if not (d[0] == "eng" and d[1] is C and not is_dma):
                    deps.append(d)
        for d in deps:
            self._wait(C, d)

    def op(self, eng, meth, reads, writes, *args, signal=True, **kw):
        C = self.eng[eng]
        self._deps(C, reads, writes, False)
        if signal:
            C.count += 1
            idx = C.count
        else:
            idx = C.count + 1
        C.ops.append(("inst", meth, args, kw, signal))
        rec = ("eng", C, idx)
        for r in reads:
            r.rd.append(rec)
        for w in writes:
            w.lw = rec
            w.rd = []

    def dma(self, q, out, in_, reads, wbuf, nowaw=False, semof=None, **kw):
        C = self.eng[q]
        sb = semof if semof is not None else wbuf
        if nowaw:
            sv = wbuf.lw
            wbuf.lw = None
            self._deps(C, reads, [wbuf], True)
            wbuf.lw = sv
        else:
            self._deps(C, reads, [wbuf], True)
        if sb.dsem is None:
            sb.dsem = self.es0.enter_context(self.nc.semaphore("d_" + sb.name))
            self.nsem += 1
            self.dmabufs.append(sb)
        sb.dval += 16
        C.ops.append(("dma", out, in_, kw, sb.dsem))
        rec = ("dma", sb.dsem, sb.dval)
        for r in reads:
            r.rd.append(rec)
        wbuf.lw = rec
        wbuf.rd = []

    def barrier(self):
        comp = [self.eng[n] for n in ("pe", "act", "dve", "pool")]
        for C in self.eng.values():
            for E in comp:
                if E is not C and E.count > 0:
                    self._wait(C, ("eng", E, E.count))
            for b in self.dmabufs:
                self._wait(C, ("dma", b.dsem, b.dval))

    def emit(self):
        nc = self.nc
        C = self.eng["sp"]
        for b in self.dmabufs:
            self._wait(C, ("dma", b.dsem, b.dval))
        hwmap = {"pe": "tensor", "act": "scalar", "dve": "vector", "pool": "gpsimd", "sp": "sync"}
        with nc.Block() as block:
            for n, E in self.eng.items():
                def body(hw, E=E):
                    for o in E.ops:
                        if o[0] == "wait":
                            hw.wait_ge(o[1], o[2])
                        elif o[0] == "inst":
                            ins = getattr(hw, o[1])(*o[2], **o[3])
                            if o[4]:
                                ins.then_inc(E.sem, 1)
                        else:
                            hw.dma_start(out=o[1], in_=o[2], **o[3]).then_inc(o[4], 16)
                getattr(block, hwmap[n])(body)


D = 2048
NB = 8
NCORE = 8
HC = 144
NCOL = NB * HC
EPS = 1e-6
WINS = (2, 4, 8, 16)


def make_consts(c, nc):
    K = {}
    idt, idb = c.sbuf("ident", [128, 128], BF16)
    c.op("pool", "memset", [], [idb], idt[:], 0.0)
    c.op("pool", "affine_select", [idb], [idb], out=idt[:], in_=idt[:], pattern=[[-1, 128]],
         compare_op=ALU.not_equal, fill=1.0, base=0, channel_multiplier=1)
    K["ident"] = (idt, idb)
    return K


def mod_step(c, nc, ccol_d, modw_d, modb_d, g_d, ps, outs, gbc_pair, mbb, ws):
    ccol, ccb = c.sbuf("ccol", [128, 16], F32)
    cond, condb = c.sbuf("cond", [128, 16], F32)
    crep, crepb = c.sbuf("crep", [128, 16, 128], BF16)
    gbc, gbcb = gbc_pair
    c.dma("sp", ccol[:], ccol_d, [], ccb)
    c.op("act", "activation", [ccb], [condb], out=cond[:], in_=ccol[:], func=AF.Silu)
    c.op("dve", "tensor_copy", [condb], [crepb], out=crep[:],
         in_=cond[:].unsqueeze(2).to_broadcast([128, 16, 128]))
    c.dma("sp", gbc[:], g_d.partition_broadcast(128), [], gbcb)
    mw = modw_d.rearrange("(kc p) n -> p kc n", p=128)
    names = ["shift", "geff", "gate"]
    for nch in range(12):
        wt, wb = ws[nch % 2]
        bt, bb = mbb[nch % 2]
        wt = wt[:].rearrange("p (k n) -> p k n", k=16)
        c.dma("pool", wt, mw[:, :, nch * 512:(nch + 1) * 512], [], wb)
        c.dma("sp", bt[:], modb_d[:, nch * 512:(nch + 1) * 512].partition_broadcast(128), [], bb)
        pt, pb = ps[nch % 2]
        for kc in range(16):
            c.op("pe", "matmul", [crepb, wb], [pb], pt[:], crep[:, kc, :], wt[:, kc, :],
                 start=(kc == 0), stop=(kc == 15), signal=(kc == 15))
        ot, ob = outs[names[nch // 4]]
        sl = slice((nch % 4) * 512, (nch % 4 + 1) * 512)
        c.op("dve", "tensor_tensor", [pb, bb], [ob], out=ot[:, sl], in0=pt[:], in1=bt[:], op=ALU.add)
    ot, ob = outs["geff"]
    c.op("dve", "scalar_tensor_tensor", [ob, gbcb], [ob], out=ot[:], in0=ot[:], scalar=1.0, in1=gbc[:],
         op0=ALU.add, op1=ALU.mult)


def norm_rows(c, xt, xb, outs, tmp, hb_t, hb_b):
    sq, sqb = tmp["sq"]
    ss, ssb = tmp["ss"]
    rs, rsb = tmp["rs"]
    h1, h1b = tmp["h1"]
    c.op("act", "activation", [xb], [sqb, ssb], out=sq[:], in_=xt, func=AF.Square, accum_out=ss[:])
    c.op("dve", "tensor_scalar", [ssb], [rsb], out=rs[:], in0=ss[:], scalar1=1.0 / D, scalar2=EPS,
         op0=ALU.mult, op1=ALU.add)
    c.op("act", "activation", [rsb], [rsb], out=rs[:], in_=rs[:], func=AF.Sqrt)
    c.op("dve", "reciprocal", [rsb], [rsb], out=rs[:], in_=rs[:])
    gt, gb = outs["geff"]
    st, sb = outs["shift"]
    c.op("dve", "scalar_tensor_tensor", [xb, rsb, gb], [h1b], out=h1[:], in0=xt, scalar=rs[:, 0:1], in1=gt[:],
         op0=ALU.mult, op1=ALU.mult)
    c.op("pool", "tensor_tensor", [h1b, sb], [hb_b], out=hb_t, in0=h1[:], in1=st[:], op=ALU.add)


def build_pool(stop=None):
    nc = bass.Bass("TRN2", target_bir_lowering=False)
    dt = nc.dram_tensor
    x_d = dt("xs", [NB, 128, D], F32, kind="ExternalInput").ap()
    xh_d = dt("xh", [128, D], F32, kind="ExternalInput").ap()
    tok_d = dt("tokrow", [1, NCOL], F32, kind="ExternalInput").ap()
    ccol_d = dt("ccol", [128, 16], F32, kind="ExternalInput").ap()
    modw_d = dt("modw", [D, 3 * D], F32, kind="ExternalInput").ap()
    modb_d = dt("modb", [1, 3 * D], F32, kind="ExternalInput").ap()
    g_d = dt("g", [1, D], F32, kind="ExternalInput").ap()
    win_d = dt("win", [D, 2 * D], F32, kind="ExternalInput").ap()
    wgrp_d = dt("wgrp", [4, 512, 512], F32, kind="ExternalInput").ap()
    ls_d = dt("lscol", [128, 16], F32, kind="ExternalInput").ap()
    wout_d = dt("wout", [D, D], F32, kind="ExternalInput").ap()
    xo_d = dt("xo", [NB, 128, D], F32, kind="ExternalOutput").ap()
    with ExitStack() as es:
        c = Ctx(nc, es)
        K = make_consts(c, nc)
        idt, idb = K["ident"]
        ps = [c.psum(f"ps{i}", [128, 512], F32) for i in range(8)]
        pTs = [(ps[6][0][:].bitcast(BF16), ps[6][1]), (ps[7][0][:].bitcast(BF16), ps[7][1])]
        outs = {n: c.sbuf(n + "_bc", [128, D], F32) for n in ("shift", "geff", "gate")}
        wsl = [c.sbuf(f"wsl{i}", [128, 16 * 512], BF16) for i in range(3)]
        xin = [c.sbuf(f"xin{i}", [128, D], F32) for i in range(2)]
        h1p = c.sbuf("h1", [128, D], F32)
        gbcp = c.sbuf("gbc", [128, D], F32)
        xo = [c.sbuf(f"xo{i}", [128, 512], F32) for i in range(2)]
        xr = [c.sbuf(f"xr{i}", [128, 512], F32) for i in range(2)]
        mod_step(c, nc, ccol_d, modw_d, modb_d, g_d, ps, outs, gbcp, xr, wsl)
        if stop == "mod":
            dbg_d = dt("dbg", [3, 128, D], F32, kind="ExternalOutput").ap()
            dbb = c.buf("dbg"); c.outbufs.append(dbb)
            for i, n in enumerate(("shift", "geff", "gate")):
                c.dma("sp", dbg_d[i], outs[n][0][:], [outs[n][1]], dbb)
            c.emit()
            return nc

        hT, hTb = c.sbuf("hT", [128, 16, NCOL], BF16)
        hbs = [c.sbuf(f"hb{i}", [128, D], BF16) for i in range(2)]
        tmp = {"sq": c.sbuf("sq", [128, D], BF16), "ss": c.sbuf("ss", [128, 1], F32),
               "rs": c.sbuf("rs", [128, 1], F32), "h1": h1p}
        for rb in range(NB + 1):
            xt, xb = xin[rb % 2]
            c.dma("sp", xt[:], x_d[rb] if rb < NB else xh_d, [], xb)
            ht, hb = hbs[rb % 2]
            norm_rows(c, xt[:], xb, outs, tmp, ht[:], hb)
            for q in range(4):
                half = q % 2
                pT, pb = pTs[half]
                for f in range(4):
                    fc = q * 4 + f
                    c.op("pe", "transpose", [hb, idb], [pb], pT[:, f * 128:(f + 1) * 128],
                         ht[:, fc * 128:(fc + 1) * 128], idt[:], signal=(f == 3))
                src = pT[:, 0:512]
                if rb < NB:
                    dst = hT[:, q * 4:(q + 1) * 4, rb * HC + 16: rb * HC + 144]
                    srcv = src.rearrange("p (f t) -> p f t", f=4)
                else:
                    dst = hT[:, q * 4:(q + 1) * 4, :].rearrange("p f (b c) -> p f b c", c=HC)[:, :, :, 0:16]
                    srcv = src.rearrange("p (f b r) -> p f b r", f=4, b=NB)
                eng = "act" if q % 2 == 0 else "dve"
                if eng == "act":
                    c.op("act", "activation", [pb], [hTb], out=dst, in_=srcv, func=AF.Copy)
                else:
                    c.op("dve", "tensor_copy", [pb], [hTb], out=dst, in_=srcv)

        if stop == "N":
            dbg_d = dt("dbg", [128, 16 * NCOL], BF16, kind="ExternalOutput").ap()
            dbb = c.buf("dbg"); c.outbufs.append(dbb)
            c.dma("sp", dbg_d, hT[:].rearrange("p k c -> p (k c)"), [hTb], dbb)
            c.emit()
            return nc
        tokbc, tokb = c.sbuf("tokbc", [128, NCOL], F32)
        c.dma("sp", tokbc[:], tok_d.partition_broadcast(128), [], tokb)
        vmask, vmb = c.sbuf("vmask", [128, NB, 16], F32)
        tok3 = tokbc[:].rearrange("p (b c) -> p b c", c=HC)
        c.op("dve", "tensor_scalar", [tokb], [vmb], out=vmask[:], in0=tok3[:, :, 0:16], scalar1=0.0, scalar2=None,
             op0=ALU.is_ge)
        cnt, cntb = c.sbuf("cnt", [128, NB, 128], F32)
        lscol, lsb = c.sbuf("lscol", [128, 16], F32)
        c.dma("sp", lscol[:], ls_d, [], lsb)
        wgs = [c.sbuf(f"wgs{i}", [128, 4, 512], BF16) for i in range(2)]
        def v3(pair):
            return (pair[0][:, 0:NCOL].rearrange("p (b c) -> p b c", c=HC), pair[1])
        vs_ = [v3(xin[0]), v3(xin[1])]
        lv = [v3(h1p), v3(gbcp)]
        pooled, pooledb = c.sbuf("pooled", [128, 4, NB * 128], BF16)
        sg = [c.sbuf(f"sg{i}", [128, 512], F32) for i in range(2)]
        uT = [(outs[n][0][:].bitcast(BF16).rearrange("p (k t) -> p k t", k=4), outs[n][1]) for n in ("shift", "geff")]
        xreg = {(b, mq): c.buf(f"xreg{b}_{mq}") for b in range(NB) for mq in range(2)}
        c.outbufs += list(xreg.values())
        winv = win_d.rearrange("(kc p) n -> p kc n", p=128)
        woutv = wout_d.rearrange("(cc p) m -> p cc m", p=128)
        wslot_i = 0
        xcount = 0
        for gi in range(4):
            w = WINS[gi]
            c.op("dve", "tensor_scalar", [tokb], [cntb], out=cnt[:], in0=tok3[:, :, 16:144], scalar1=1.0,
                 scalar2=float(w), op0=ALU.add, op1=ALU.min)
            c.op("dve", "reciprocal", [cntb], [cntb], out=cnt[:], in_=cnt[:])
            Wv_t, Wv_b = wsl[wslot_i % 3]; wslot_i += 1
            Wv = Wv_t[:].rearrange("p (k n) -> p k n", k=16)
            c.dma("pool", Wv, winv[:, :, gi * 512:(gi + 1) * 512], [], Wv_b)
            Wg_t, Wg_b = wsl[wslot_i % 3]; wslot_i += 1
            Wg = Wg_t[:].rearrange("p (k n) -> p k n", k=16)
            c.dma("pool", Wg, winv[:, :, D + gi * 512: D + (gi + 1) * 512], [], Wg_b)
            wgt, wgb = wgs[gi % 2]
            c.dma("pool", wgt[:], wgrp_d[gi].rearrange("(cc p) n -> p cc n", p=128), [], wgb)
            for fl in range(4):
                for kc in range(16):
                    for j in range(3):
                        pt, pb = ps[j]
                        c.op("pe", "matmul", [Wv_b, hTb], [pb], pt[:, 0:384], Wv[:, kc, fl * 128:(fl + 1) * 128],
                             hT[:, kc, j * 384:(j + 1) * 384], start=(kc == 0), stop=(kc == 15),
                             signal=(kc == 15))
                vt, vb = vs_[fl % 2]
                vflat = xin[fl % 2][0][:, 0:NCOL]
                for j in range(3):
                    pt, pb = ps[j]
                    c.op("act", "activation", [pb], [vb], out=vflat[:, j * 384:(j + 1) * 384], in_=pt[:, 0:384],
                         func=AF.Copy)
                c.op("dve", "tensor_tensor", [vb, vmb], [vb], out=vt[:, :, 0:16], in0=vt[:, :, 0:16], in1=vmask[:],
                     op=ALU.mult)
                cur, curb = vt, vb
                off = 0
                step = 1
                li = 0
                while step < w:
                    off += step
                    nt, nb_ = lv[li % 2]
                    li += 1
                    c.op("dve", "tensor_tensor", [curb], [nb_], out=nt[:, :, off:HC], in0=cur[:, :, off:HC],
                         in1=cur[:, :, off - step:HC - step], op=ALU.add)
                    cur, curb = nt, nb_
                    step *= 2
                nt, nb_ = lv[li % 2]
                c.op("dve", "tensor_tensor", [curb, cntb], [nb_], out=nt[:, :, 16:HC], in0=cur[:, :, 16:HC],
                     in1=cnt[:], op=ALU.mult)
                c.op("pool", "tensor_tensor", [nb_, vb], [pooledb],
                     out=pooled[:, fl, :].rearrange("p (b t) -> p b t", b=NB), in0=nt[:, :, 16:HC],
                     in1=vt[:, :, 16:HC], op=ALU.subtract)
            ut, ub = uT[gi % 2]
            for nl in range(4):
                nchunk = gi * 4 + nl
                for j in range(2):
                    pm, pmb = ps[3 + j]
                    for cc in range(4):
                        c.op("pe", "matmul", [wgb, pooledb], [pmb], pm[:], wgt[:, cc, nl * 128:(nl + 1) * 128],
                             pooled[:, cc, j * 512:(j + 1) * 512], start=(cc == 0), stop=(cc == 3),
                             signal=(cc == 3))
                    pg, pgb = ps[5 + j]
                    for kc in range(16):
                        rhs = hT[:, kc, :].rearrange("p (b c) -> p b c", c=HC)[:, j * 4:(j + 1) * 4, 16:HC]
                        c.op("pe", "matmul", [Wg_b, hTb], [pgb], pg[:], Wg[:, kc, nl * 128:(nl + 1) * 128], rhs,
                             start=(kc == 0), stop=(kc == 15), signal=(kc == 15))
                    st_, sb_ = sg[j]
                    c.op("act", "activation", [pgb], [sb_], out=st_[:], in_=pg[:], func=AF.Silu)
                    c.op("dve", "scalar_tensor_tensor", [pmb, lsb, sb_], [ub], out=ut[:, nl, j * 512:(j + 1) * 512],
                         in0=pm[:], scalar=lscol[:, nchunk:nchunk + 1], in1=st_[:], op0=ALU.mult, op1=ALU.mult)
            gt_, gb_ = outs["gate"]
            for mh in range(2):
                Wo_t, Wo_b = wsl[wslot_i % 3]; wslot_i += 1
                Wo = Wo_t[:, 0:4096].rearrange("p (k m) -> p k m", k=4)
                c.dma("pool", Wo, woutv[:, gi * 4:(gi + 1) * 4, mh * 1024:(mh + 1) * 1024], [], Wo_b)
                for b in range(NB):
                    for mq in range(2):
                        mcol = mh * 1024 + mq * 512
                        po, pob = ps[(b * 2 + mq) % 3]
                        for cc in range(4):
                            c.op("pe", "matmul", [ub, Wo_b], [pob], po[:], ut[:, cc, b * 128:(b + 1) * 128],
                                 Wo[:, cc, mq * 512:(mq + 1) * 512], start=(cc == 0), stop=(cc == 3),
                                 signal=(cc == 3))
                        xrt, xrb = xr[xcount % 2]
                        xot, xob = xo[xcount % 2]
                        xcount += 1
                        src = x_d if gi == 0 else xo_d
                        c.dma("sp", xrt[:], src[b, :, mcol:mcol + 512], [xreg[(b, mq)]] if gi > 0 else [], xrb)
                        c.op("dve", "tensor_tensor", [pob, gb_], [xob], out=xot[:], in0=po[:],
                             in1=gt_[:, mcol:mcol + 512], op=ALU.mult)
                        c.op("pool", "tensor_tensor", [xob, xrb], [xob], out=xot[:], in0=xot[:], in1=xrt[:],
                             op=ALU.add)
                        c.dma("sp", xo_d[b, :, mcol:mcol + 512], xot[:], [xob], xreg[(b, mq)])
        c.emit()
    return nc


ATT_IN = 9296
NH = 16
TWO_PI = 6.283185307179586


def rope_tables(c, pos_d, invf_d):
    posi, posib = c.sbuf("posi", [128, NB], I32)
    posf, posfb = c.sbuf("posf", [128, NB], F32)
    invf, invfb = c.sbuf("invf", [128, 24], F32)
    cs, csb = c.sbuf("cs", [128, NB, 2, 24], F32)
    yk, ykb = c.sbuf("yk", [128, NB, 2, 24], I32)
    yf, yfb = c.sbuf("yf", [128, NB, 2, 24], F32)
    m1, m1b = c.sbuf("rm1", [128, NB, 2, 24], F32)
    c.dma("sp", posi[:], pos_d, [], posib)
    c.dma("sp", invf[:], invf_d.partition_broadcast(128), [], invfb)
    c.op("dve", "tensor_copy", [posib], [posfb], out=posf[:], in_=posi[:])
    for j in range(NB):
        c.op("dve", "tensor_scalar", [posfb, invfb], [csb], out=cs[:, j, 1, :], in0=invf[:], scalar1=posf[:, j:j + 1],
             scalar2=1.0 / TWO_PI, op0=ALU.mult, op1=ALU.mult)
    c.op("dve", "tensor_scalar", [csb], [csb], out=cs[:, :, 0, :], in0=cs[:, :, 1, :], scalar1=0.25, scalar2=None,
         op0=ALU.add)
    c.op("dve", "tensor_copy", [csb], [ykb], out=yk[:], in_=cs[:])
    c.op("dve", "tensor_copy", [ykb], [yfb], out=yf[:], in_=yk[:])
    c.op("dve", "tensor_tensor", [csb, yfb], [csb], out=cs[:], in0=cs[:], in1=yf[:], op=ALU.subtract)
    c.op("dve", "tensor_scalar", [csb], [m1b], out=m1[:], in0=cs[:], scalar1=0.5, scalar2=None, op0=ALU.is_gt)
    c.op("dve", "tensor_tensor", [csb, m1b], [csb], out=cs[:], in0=cs[:], in1=m1[:], op=ALU.subtract)
    c.op("dve", "tensor_scalar", [csb], [m1b], out=m1[:], in0=cs[:], scalar1=-0.5, scalar2=None, op0=ALU.is_lt)
    c.op("dve", "tensor_tensor", [csb, m1b], [csb], out=cs[:], in0=cs[:], in1=m1[:], op=ALU.add)
    c.op("act", "activation", [csb], [csb], out=cs[:], in_=cs[:], func=AF.Sin, scale=TWO_PI)
    return cs, csb


def build_a1(stop=None):
    nc = bass.Bass("TRN2", target_bir_lowering=False)
    dt = nc.dram_tensor
    x_d = dt("xs", [NB, 128, D], F32, kind="ExternalInput").ap()
    ccol_d = dt("ccol", [128, 16], F32, kind="ExternalInput").ap()
    modw_d = dt("modw", [D, 3 * D], F32, kind="ExternalInput").ap()
    modb_d = dt("modb", [1, 3 * D], F32, kind="ExternalInput").ap()
    g_d = dt("g", [1, D], F32, kind="ExternalInput").ap()
    win_d = dt("win", [D, ATT_IN], F32, kind="ExternalInput").ap()
    pos_d = dt("posc", [128, NB], I32, kind="ExternalInput").ap()
    invf_d = dt("invf", [1, 24], F32, kind="ExternalInput").ap()
    QT_d = dt("QT", [NH, 128, NB * 128], BF16, kind="ExternalOutput").ap()
    KT_d = dt("KT", [NH, 128, NB * 128], BF16, kind="ExternalOutput").ap()
    SG_d = dt("SG", [NH, 128, NB * 128], BF16, kind="ExternalOutput").ap()
    V_d = dt("V", [NB * 128, D], BF16, kind="ExternalOutput").ap()
    QI_d = dt("QIT", [8, 128, NB * 128], BF16, kind="ExternalOutput").ap()
    KI_d = dt("KIT", [64, NB * 128], BF16, kind="ExternalOutput").ap()
    WI_d = dt("WI", [128, NB, 16], F32, kind="ExternalOutput").ap()
    GB_d = dt("GBC", [128, D], F32, kind="ExternalOutput").ap()
    with ExitStack() as es:
        c = Ctx(nc, es)
        K = make_consts(c, nc)
        idt, idb = K["ident"]
        ps = [c.psum(f"ps{i}", [128, 512], F32) for i in range(8)]
        pTs = [(ps[6][0][:].bitcast(BF16), ps[6][1]), (ps[7][0][:].bitcast(BF16), ps[7][1])]
        outs = {n: c.sbuf(n + "_bc", [128, D], F32) for n in ("shift", "geff", "gate")}
        wsl = [c.sbuf(f"wsl{i}", [128, 16 * 512], BF16) for i in range(3)]
        xin = [c.sbuf(f"xin{i}", [128, D], F32) for i in range(2)]
        h1p = c.sbuf("h1", [128, D], F32)
        gbcp = c.sbuf("gbc", [128, D], F32)
        xr = [c.sbuf(f"xr{i}", [128, 512], F32) for i in range(2)]
        mod_step(c, nc, ccol_d, modw_d, modb_d, g_d, ps, outs, gbcp, xr, wsl)
        ob = {n: c.buf("o_" + n) for n in ("QT", "KT", "SG", "V", "QI", "KI", "WI", "GB")}
        c.outbufs += list(ob.values())
        c.dma("sp", GB_d, outs["gate"][0][:], [outs["gate"][1]], ob["GB"])
        cs, csb = rope_tables(c, pos_d, invf_d)
        if stop == "rope":
            dbg_d = dt("dbg", [128, NB * 48], F32, kind="ExternalOutput").ap()
            c.dma("sp", dbg_d, cs[:].rearrange("p a b f -> p (a b f)"), [csb], c.buf("dbg"))
            c.emit()
            return nc
        hT, hTb = c.sbuf("hT", [128, 16, NB * 128], BF16)
        hbs = [c.sbuf(f"hb{i}", [128, D], BF16) for i in range(2)]
        tmp = {"sq": c.sbuf("sq", [128, D], BF16), "ss": c.sbuf("ss", [128, 1], F32),
               "rs": c.sbuf("rs", [128, 1], F32), "h1": h1p}
        for rb in range(NB):
            xt, xb = xin[rb % 2]
            c.dma("sp", xt[:], x_d[rb], [], xb)
            ht, hb = hbs[rb % 2]
            norm_rows(c, xt[:], xb, outs, tmp, ht[:], hb)
            for q in range(4):
                pT, pb = pTs[q % 2]
                for f in range(4):
                    fc = q * 4 + f
                    c.op("pe", "transpose", [hb, idb], [pb], pT[:, f * 128:(f + 1) * 128],
                         ht[:, fc * 128:(fc + 1) * 128], idt[:], signal=(f == 3))
                dst = hT[:, q * 4:(q + 1) * 4, rb * 128:(rb + 1) * 128]
                srcv = pT[:, 0:512].rearrange("p (f t) -> p f t", f=4)
                if q % 2 == 0:
                    c.op("act", "activation", [pb], [hTb], out=dst, in_=srcv, func=AF.Copy)
                else:
                    c.op("dve", "tensor_copy", [pb], [hTb], out=dst, in_=srcv)
        cexp = []
        for nm, nh_e, half_e, f0_e in (("cq", 4, 16, 0), ("ci", 8, 8, 16)):
            ce, ceb = c.sbuf(nm, [128, NB, 2, 64], F32)
            for b in range(NB):
                for t_ in range(2):
                    c.op("dve", "tensor_copy", [csb], [ceb], out=ce[:, b, t_, :].rearrange("p (h d) -> p h d", h=nh_e),
                         in_=cs[:, b, t_, f0_e:f0_e + half_e].unsqueeze(1).to_broadcast([128, nh_e, half_e]))
            cexp.append((ce, ceb))
        winv = win_d.rearrange("(kc p) n -> p kc n", p=128)
        tms = [c.sbuf(f"tm{i}", [128, 512], BF16) for i in range(2)]
        xfs = [c.sbuf(f"xf{i}", [128, 512], F32) for i in range(2)]
        stg = [c.sbuf(f"stg{i}", [128, 512], BF16) for i in range(2)]
        rt = [c.sbuf(f"rt{i}", [128, 64], F32) for i in range(4)]
        wif = [c.sbuf(f"wif{i}", [128, 16], F32) for i in range(2)]
        chunks = list(range(19)) if stop is None else stop
        it = 0
        for ch in chunks:
            c0 = ch * 512
            ncol = 512 if ch < 18 else ATT_IN - 18 * 512
            Wt, Wb = wsl[ch % 3]
            W = Wt[:, 0:16 * ncol].rearrange("p (k n) -> p k n", k=16)
            c.dma("pool", W, winv[:, :, c0:c0 + ncol], [], Wb)
            kind = ("q", "k", "v", "g", "qi")[ch // 4] if ch < 18 else "ki"
            for b in range(NB):
                pt, pb = ps[it % 4]
                tm, tmb = tms[it % 2]
                sg_, sgb = stg[it % 2]
                pT, pTb_ = pTs[it % 2]
                it += 1
                for kc in range(16):
                    c.op("pe", "matmul", [hTb, Wb], [pb], pt[:, 0:ncol], hT[:, kc, b * 128:(b + 1) * 128], W[:, kc, :],
                         start=(kc == 0), stop=(kc == 15), signal=(kc == 15))
                tsl = slice(b * 128, (b + 1) * 128)
                if kind == "v":
                    c.op("act", "activation", [pb], [tmb], out=tm[:], in_=pt[:], func=AF.Copy)
                    c.dma("sp", V_d[tsl, (ch - 8) * 512:(ch - 7) * 512], tm[:], [tmb], ob["V"], nowaw=True, semof=tmb)
                    continue
                if kind == "g":
                    c.op("act", "activation", [pb], [tmb], out=tm[:], in_=pt[:], func=AF.Silu)
                else:
                    if kind in ("q", "k"):
                        nh_, hd, half, f0 = 4, 128, 16, 0
                    elif kind == "qi":
                        nh_, hd, half, f0 = 8, 64, 8, 16
                    else:
                        nh_, hd, half, f0 = 1, 64, 8, 16
                    wcol = nh_ * hd
                    xf_, xfb_ = xfs[it % 2]
                    c.op("act", "activation", [pb], [xfb_], out=xf_[:, 0:wcol], in_=pt[:, 0:wcol], func=AF.Copy)
                    c.op("act", "activation", [pb], [tmb], out=tm[:, 0:wcol], in_=pt[:, 0:wcol], func=AF.Copy)
                    pv = xf_[:, 0:wcol].rearrange("p (h d) -> p h d", h=nh_)
                    tv = tm[:, 0:wcol].rearrange("p (h d) -> p h d", h=nh_)
                    rsb_ = xfb_
                    ceb_ = csb
                    if kind == "ki":
                        cosb = cs[:, b, 0, f0:f0 + half].unsqueeze(1)
                        sinb = cs[:, b, 1, f0:f0 + half].unsqueeze(1)
                    else:
                        ce, ceb_ = cexp[0] if kind in ("q", "k") else cexp[1]
                        cosb = ce[:, b, 0, :].rearrange("p (h d) -> p h d", h=nh_)
                        sinb = ce[:, b, 1, :].rearrange("p (h d) -> p h d", h=nh_)
                    x1 = pv[:, :, 0:half]
                    x2 = pv[:, :, half:2 * half]
                    tt = [(rt[i][0][:, 0:nh_ * half].rearrange("p (h d) -> p h d", h=nh_), rt[i][1]) for i in range(4)]
                    c.op("dve", "tensor_tensor", [rsb_, ceb_], [tt[0][1]], out=tt[0][0], in0=x1, in1=cosb, op=ALU.mult,
                         signal=False)
                    c.op("dve", "tensor_tensor", [rsb_, ceb_], [tt[1][1]], out=tt[1][0], in0=x2, in1=sinb, op=ALU.mult,
                         signal=False)
                    c.op("dve", "tensor_tensor", [rsb_, ceb_], [tt[2][1]], out=tt[2][0], in0=x2, in1=cosb, op=ALU.mult,
                         signal=False)
                    c.op("dve", "tensor_tensor", [rsb_, ceb_], [tt[3][1]], out=tt[3][0], in0=x1, in1=sinb, op=ALU.mult)
                    c.op("dve", "tensor_tensor", [tt[0][1], tt[1][1]], [tmb], out=tv[:, :, 0:half], in0=tt[0][0],
                         in1=tt[1][0], op=ALU.subtract, signal=False)
                    c.op("dve", "tensor_tensor", [tt[2][1], tt[3][1]], [tmb], out=tv[:, :, half:2 * half], in0=tt[2][0],
                         in1=tt[3][0], op=ALU.add)
                    if kind == "ki":
                        wt_, wb_ = wif[b % 2]
                        c.op("act", "activation", [pb], [wb_], out=wt_[:], in_=pt[:, 64:80], func=AF.Copy)
                        c.dma("sp", WI_d[:, b, :], wt_[:], [wb_], ob["WI"], nowaw=True, semof=wb_)
                if kind == "ki":
                    c.op("pe", "transpose", [tmb, idb], [pTb_], pT[:, 0:128], tm[:, 0:128], idt[:])
                    c.op("dve", "tensor_copy", [pTb_], [sgb], out=sg_[0:64, 0:128], in_=pT[0:64, 0:128])
                    c.dma("sp", KI_d[:, tsl], sg_[0:64, 0:128], [sgb], ob["KI"], nowaw=True, semof=sgb)
                    continue
                for f in range(4):
                    c.op("pe", "transpose", [tmb, idb], [pTb_], pT[:, f * 128:(f + 1) * 128],
                         tm[:, f * 128:(f + 1) * 128], idt[:], signal=(f == 3))
                c.op("dve", "tensor_copy", [pTb_], [sgb], out=sg_[:], in_=pT[:, 0:512])
                dd, key, hc = {"q": (QT_d, "QT", ch), "k": (KT_d, "KT", ch - 4), "g": (SG_d, "SG", ch - 12),
                               "qi": (QI_d, "QI", ch - 16)}[kind]
                dst = dd[hc * 4:(hc + 1) * 4, :, tsl].rearrange("h d t -> d h t")
                c.dma("sp", dst, sg_[:].rearrange("p (h t) -> p h t", h=4), [sgb], ob[key], nowaw=True, semof=sgb)
        c.emit()
    return nc


NIT = 20
TOPK = 256
BIG = 1.0e30


def mt_base(kb):
    J = kb // 8
    return sum(8 * (8 - jj) for jj in range(J)) + (kb - 8 * J) * (8 - J)


def build_a2(stop=None):
    nc = bass.Bass("TRN2", target_bir_lowering=False)
    dt = nc.dram_tensor
    S = NB * 128 * NCORE
    x_d = dt("xs", [NB, 128, D], F32, kind="ExternalInput").ap()
    GB_d = dt("GBC", [128, D], F32, kind="ExternalInput").ap()
    QT_d = dt("QT", [NH, 128, NB * 128], BF16, kind="ExternalInput").ap()
    SG_d = dt("SG", [NH, 128, NB * 128], BF16, kind="ExternalInput").ap()
    QI_d = dt("QIT", [8, 128, NB * 128], BF16, kind="ExternalInput").ap()
    WI_d = dt("WI", [128, NB, 16], F32, kind="ExternalInput").ap()
    KT_d = dt("KTg", [NH, 128, S], BF16, kind="ExternalInput").ap()
    V_d = dt("Vg", [NH, 128, 64, 128], BF16, kind="ExternalInput").ap()
    KI_d = dt("KITg", [64, S], BF16, kind="ExternalInput").ap()
    trel_d = dt("trel", [128, 1], F32, kind="ExternalInput").ap()
    wout_d = dt("wout", [D, D], F32, kind="ExternalInput").ap()
    xo_d = dt("xo", [NB, 128, D], F32, kind="ExternalOutput").ap()
    with ExitStack() as es:
        c = Ctx(nc, es)
        K = make_consts(c, nc)
        idt, idb = K["ident"]
        ps = [c.psum(f"ps{i}", [128, 512], F32) for i in range(8)]
        pTs = [(ps[6][0][:].bitcast(BF16), ps[6][1]), (ps[7][0][:].bitcast(BF16), ps[7][1])]
        mT, mTb = c.sbuf("mT", [128, 288, 128], BF16)
        ones, onesb = c.sbuf("ones", [128, 128], BF16)
        c.op("pool", "memset", [], [onesb], ones[:], 1.0)
        with ExitStack() as es2:
            c.es = es2
            ki2, ki2b = c.sbuf("ki2", [128, S], BF16)
            c.dma("sp", ki2[0:64, :], KI_d, [], ki2b)
            c.dma("sp", ki2[64:128, :], KI_d, [], ki2b)
            qit, qitb = c.sbuf("qit", [128, 8, NB * 128], BF16)
            c.dma("sp", qit[:], QI_d.rearrange("c p t -> p c t"), [], qitb)
            wi, wib = c.sbuf("wi", [128, NB, 16], F32)
            c.dma("sp", wi[:], WI_d, [], wib)
            trel, trelb = c.sbuf("trel", [128, 1], F32)
            c.dma("sp", trel[:], trel_d, [], trelb)
            ioi, ioib = c.sbuf("ioi", [128, 1024], I32)
            cm, cmb = c.sbuf("cm", [128, 1024], F32)
            nbias, nbb = c.sbuf("nbias", [128, 1024], F32)
            c.op("pool", "iota", [], [ioib], ioi[:], pattern=[[1, 1024]], base=0, channel_multiplier=0)
            c.op("dve", "tensor_copy", [ioib], [cmb], out=cm[:], in_=ioi[:])
            c.op("dve", "tensor_scalar", [cmb, trelb], [cmb], out=cm[:], in0=cm[:], scalar1=trel[:, 0:1], scalar2=None,
                 op0=ALU.is_le)
            c.op("dve", "tensor_scalar", [cmb], [nbb], out=nbias[:], in0=cm[:], scalar1=-1.0, scalar2=BIG,
                 op0=ALU.add, op1=ALU.mult)
            p2, p2b = c.sbuf("pow2", [128, NIT + 2], F32)
            for k in range(NIT + 2):
                c.op("pool", "memset", [], [p2b], p2[:, k:k + 1], float(2.0 ** (-k)))
            Sc, Scb = c.sbuf("Sc", [128, S], F32)
            mk, mkb = c.sbuf("mk", [128, S], BF16)
            rr = [c.sbuf(f"rr{i}", [128, 512], F32) for i in range(4)]
            accB, accBb = c.sbuf("accB", [128, 512], F32)
            sm = {n: c.sbuf("bs_" + n, [128, 1], F32) for n in ("mn", "mx", "W", "cand", "cnt", "u", "thr")}
            steps, stepsb = c.sbuf("steps", [128, NIT + 2], F32)
            it = 0
            for j in range(NB):
                n = 1024 * (j + 1)
                tsl = slice(j * 128, (j + 1) * 128)
                for kt in range(2 * (j + 1)):
                    ksl = slice(kt * 512, (kt + 1) * 512)
                    for h in range(16):
                        par = h % 2
                        psl = slice(par * 64, (par + 1) * 64)
                        pt, pb = ps[it % 6]
                        rt_, rb_ = rr[it % 4]
                        it += 1
                        c.op("pe", "matmul", [qitb, ki2b], [pb], pt[:], qit[psl, h // 2, tsl], ki2[psl, ksl],
                             start=True, stop=True)
                        c.op("act", "activation", [pb], [rb_], out=rt_[:], in_=pt[:], func=AF.Relu)
                        eng, at, ab = "dve", Sc[:, ksl], Scb
                        if h < 1:
                            c.op(eng, "tensor_scalar", [rb_, wib], [ab], out=at, in0=rt_[:], scalar1=wi[:, j, h:h + 1],
                                 scalar2=None, op0=ALU.mult)
                        elif eng == "dve":
                            c.op(eng, "scalar_tensor_tensor", [rb_, wib, ab], [ab], out=at, in0=rt_[:],
                                 scalar=wi[:, j, h:h + 1], in1=at, op0=ALU.mult, op1=ALU.add)
                        else:
                            c.op(eng, "tensor_scalar", [rb_, wib], [rb_], out=rt_[:], in0=rt_[:],
                                 scalar1=wi[:, j, h:h + 1], scalar2=None, op0=ALU.mult)
                            c.op(eng, "tensor_tensor", [rb_, ab], [ab], out=at, in0=at, in1=rt_[:], op=ALU.add)
                mn, mnb = sm["mn"]; mx, mxb = sm["mx"]; W_, Wb_ = sm["W"]; cand, candb = sm["cand"]
                cnt, cntb = sm["cnt"]; u_, ub_ = sm["u"]; thr, thrb = sm["thr"]
                c.op("dve", "tensor_reduce", [Scb], [mnb], out=mn[:], in_=Sc[:, 0:n], axis=AX.X, op=ALU.min)
                c.op("dve", "tensor_reduce", [Scb], [mxb], out=mx[:], in_=Sc[:, 0:n], axis=AX.X, op=ALU.max)
                c.op("dve", "tensor_tensor", [mxb, mnb], [Wb_], out=W_[:], in0=mx[:], in1=mn[:], op=ALU.subtract)
                c.op("dve", "tensor_scalar", [Wb_, p2b], [stepsb], out=steps[:], in0=p2[:], scalar1=W_[:, 0:1], scalar2=None,
                     op0=ALU.mult)
                c.op("dve", "tensor_tensor", [mnb, stepsb], [candb], out=cand[:], in0=mn[:], in1=steps[:, 1:2],
                     op=ALU.add)
                lsl = slice(n - 1024, n)
                c.op("dve", "tensor_tensor", [Scb, cmb], [Scb], out=Sc[:, lsl], in0=Sc[:, lsl], in1=cm[:], op=ALU.mult)
                c.op("dve", "tensor_tensor", [Scb, nbb], [Scb], out=Sc[:, lsl], in0=Sc[:, lsl], in1=nbias[:], op=ALU.add)
                for k in range(1, NIT + 1):
                    c.op("dve", "tensor_scalar", [Scb, candb], [mkb, cntb], out=mk[:, 0:n], in0=Sc[:, 0:n],
                         scalar1=cand[:, 0:1], scalar2=None, op0=ALU.is_ge, op1=ALU.add, accum_out=cnt[:])
                    c.op("dve", "tensor_scalar", [cntb], [ub_], out=u_[:], in0=cnt[:], scalar1=float(TOPK) - 0.5,
                         scalar2=0.5, op0=ALU.is_ge, op1=ALU.subtract)
                    c.op("dve", "scalar_tensor_tensor", [ub_, stepsb, candb], [candb], out=cand[:], in0=u_[:],
                         scalar=steps[:, k:k + 1], in1=cand[:], op0=ALU.mult, op1=ALU.add)
                c.op("dve", "scalar_tensor_tensor", [stepsb, candb], [thrb], out=thr[:], in0=steps[:, NIT + 1:NIT + 2],
                     scalar=-2.0, in1=cand[:], op0=ALU.mult, op1=ALU.add)
                c.op("dve", "tensor_scalar", [Scb, thrb], [mkb], out=mk[:, 0:n], in0=Sc[:, 0:n], scalar1=thr[:, 0:1],
                     scalar2=None, op0=ALU.is_ge)
                for g4 in range(2 * (j + 1)):
                    kb0 = g4 * 4
                    J = kb0 // 8
                    pT, pTb_ = pTs[g4 % 2]
                    for f in range(4):
                        kb = kb0 + f
                        c.op("pe", "transpose", [mkb, idb], [pTb_], pT[:, f * 128:(f + 1) * 128],
                             mk[:, kb * 128:(kb + 1) * 128], idt[:], signal=(f == 3))
                    b0 = mt_base(8 * J)
                    dst = mT[:, b0:b0 + 8 * (8 - J), :].rearrange("p (kb jj) t -> p kb jj t", jj=8 - J)[
                        :, kb0 - 8 * J:kb0 - 8 * J + 4, j - J, :]
                    src = pT[:, 0:512].rearrange("p (f t) -> p f t", f=4)
                    if g4 % 2 == 0:
                        c.op("act", "activation", [pTb_], [mTb], out=dst, in_=src, func=AF.Copy)
                    else:
                        c.op("pool", "tensor_copy", [pTb_], [mTb], out=dst, in_=src) if False else \
                            c.op("dve", "tensor_copy", [pTb_], [mTb], out=dst, in_=src)
            c.barrier()
        c.es = es
        kts = [c.sbuf(f"kts{i}", [128, S // 2], BF16) for i in range(2)]
        vts = [c.sbuf(f"vts{i}", [128, 32, 128], BF16) for i in range(2)]
        qts = [c.sbuf(f"qts{i}", [128, NB * 128], BF16) for i in range(2)]
        sgs = [c.sbuf(f"sgs{i}", [128, NB * 128], BF16) for i in range(2)]
        pex = [c.sbuf(f"pex{i}", [128, 512], BF16) for i in range(3)]
        pms = [c.sbuf(f"pms{i}", [128, 512], BF16) for i in range(3)]
        ogT, ogTb = c.sbuf("ogT", [128, NH, NB * 128], BF16)
        rl, rlb = c.sbuf("rl", [128, 512], F32)
        on_, onb = c.sbuf("on", [128, 512], F32)
        scale = float(128 ** -0.5)
        it = 0
        hh = 0
        for h in range(NH):
            qtt, qtb = qts[h % 2]
            sgt, sgb = sgs[h % 2]
            for kh in range(2):
                ktt_, ktb_ = kts[kh]
                vtt_, vtb_ = vts[kh]
                c.dma("sp", ktt_[:], KT_d[h, :, kh * (S // 2):(kh + 1) * (S // 2)], [], ktb_)
                c.dma("sp", vtt_[:], V_d[h, :, kh * 32:(kh + 1) * 32, :], [], vtb_)
                if kh == 0:
                    c.dma("sp", qtt[:], QT_d[h], [], qtb)
                    c.dma("sp", sgt[:], SG_d[h], [], sgb)
            for half in range(2):
                jlo = 4 * half
                po, pob = ps[4 + 2 * (hh % 2)]
                pl, plb = ps[5 + 2 * (hh % 2)]
                hh += 1
                nkb = 8 * (jlo + 4)
                for kb in range(nkb):
                    J = kb // 8
                    j0 = max(J, jlo)
                    nj = jlo + 4 - j0
                    N = 128 * nj
                    t0 = 128 * j0
                    pL, pLb = ps[it % 4]
                    pe_, peb = pex[it % 3]
                    pm_, pmb = pms[it % 3]
                    it += 1
                    ktt, ktb = kts[kb // 32]
                    vtt, vtb = vts[kb // 32]
                    kbl = kb % 32
                    c.op("pe", "matmul", [ktb, qtb], [pLb], pL[:, 0:N], ktt[:, kbl * 128:(kbl + 1) * 128], qtt[:, t0:t0 + N],
                         start=True, stop=True)
                    c.op("act", "activation", [pLb], [peb], out=pe_[:, 0:N], in_=pL[:, 0:N], func=AF.Exp, scale=scale)
                    mb0 = mt_base(kb) + (j0 - J)
                    eng = "dve"
                    c.op(eng, "tensor_tensor", [peb, mTb], [pmb], out=pm_[:, 0:N], in0=pe_[:, 0:N],
                         in1=mT[:, mb0:mb0 + nj, :].rearrange("p a t -> p (a t)"), op=ALU.mult)
                    osl = slice((j0 - jlo) * 128, 512)
                    st_, sp_ = (kb == 0), (kb == nkb - 1)
                    c.op("pe", "matmul", [vtb, pmb], [pob], po[:, osl], vtt[:, kbl, :], pm_[:, 0:N],
                         start=st_, stop=sp_, signal=False)
                    c.op("pe", "matmul", [onesb, pmb], [plb], pl[:, osl], ones[:], pm_[:, 0:N],
                         start=st_, stop=sp_, signal=True)
                hsl = slice(half * 512, (half + 1) * 512)
                c.op("dve", "reciprocal", [plb], [rlb], out=rl[:], in_=pl[:])
                c.op("dve", "tensor_tensor", [pob, rlb], [onb], out=on_[:], in0=po[:], in1=rl[:], op=ALU.mult)
                c.op("pool", "tensor_tensor", [onb, sgb], [ogTb], out=ogT[:, h, hsl], in0=on_[:], in1=sgt[:, hsl],
                     op=ALU.mult)
        gbc, gbcb = c.sbuf("gbc", [128, D], F32)
        c.dma("sp", gbc[:], GB_d, [], gbcb)
        wsl = [c.sbuf(f"wsl{i}", [128, 16, 512], BF16) for i in range(2)]
        xo = [c.sbuf(f"xo{i}", [128, 512], F32) for i in range(2)]
        xr = [c.sbuf(f"xr{i}", [128, 512], F32) for i in range(2)]
        xod = c.buf("xo_d")
        woutv = wout_d.rearrange("(kc p) m -> p kc m", p=128)
        it = 0
        for mc in range(4):
            Wt, Wb = wsl[mc % 2]
            msl = slice(mc * 512, (mc + 1) * 512)
            c.dma("pool", Wt[:], woutv[:, :, msl], [], Wb)
            for b in range(NB):
                po, pob = ps[it % 4]
                xrt, xrb = xr[it % 2]
                xot, xob = xo[it % 2]
                it += 1
                for hc in range(16):
                    c.op("pe", "matmul", [ogTb, Wb], [pob], po[:], ogT[:, hc, b * 128:(b + 1) * 128], Wt[:, hc, :],
                         start=(hc == 0), stop=(hc == 15), signal=(hc == 15))
                c.dma("sp", xrt[:], x_d[b, :, msl], [], xrb)
                c.op("dve", "tensor_tensor", [pob, gbcb], [xob], out=xot[:], in0=po[:], in1=gbc[:, msl], op=ALU.mult)
                c.op("pool", "tensor_tensor", [xob, xrb], [xob], out=xot[:], in0=xot[:], in1=xrt[:], op=ALU.add)
                c.dma("sp", xo_d[b, :, msl], xot[:], [xob], xod, nowaw=True, semof=xob)
        c.emit()
    return nc


def build_final():
    nc = bass.Bass("TRN2", target_bir_lowering=False)
    dt = nc.dram_tensor
    x_d = dt("xs", [NB, 128, D], F32, kind="ExternalInput").ap()
    g_d = dt("g", [1, D], F32, kind="ExternalInput").ap()
    xo_d = dt("xo", [NB, 128, D], F32, kind="ExternalOutput").ap()
    with ExitStack() as es:
        c = Ctx(nc, es)
        gbc, gbcb = c.sbuf("gbc", [128, D], F32)
        c.dma("sp", gbc[:], g_d.partition_broadcast(128), [], gbcb)
        xin = [c.sbuf(f"xin{i}", [128, D], F32) for i in range(2)]
        xo = [c.sbuf(f"xo{i}", [128, D], F32) for i in range(2)]
        sq, sqb = c.sbuf("sq", [128, D], BF16)
        ss, ssb = c.sbuf("ss", [128, 1], F32)
        rs, rsb = c.sbuf("rs", [128, 1], F32)
        xod = c.buf("xo_d")
        for b in range(NB):
            xt, xb = xin[b % 2]
            ot, ob = xo[b % 2]
            c.dma("sp", xt[:], x_d[b], [], xb)
            c.op("act", "activation", [xb], [sqb, ssb], out=sq[:], in_=xt[:], func=AF.Square, accum_out=ss[:])
            c.op("dve", "tensor_scalar", [ssb], [rsb], out=rs[:], in0=ss[:], scalar1=1.0 / D, scalar2=EPS,
                 op0=ALU.mult, op1=ALU.add)
            c.op("act", "activation", [rsb], [rsb], out=rs[:], in_=rs[:], func=AF.Sqrt)
            c.op("dve", "reciprocal", [rsb], [rsb], out=rs[:], in_=rs[:])
            c.op("dve", "scalar_tensor_tensor", [xb, rsb, gbcb], [ob], out=ot[:], in0=xt[:], scalar=rs[:, 0:1],
                 in1=gbc[:], op0=ALU.mult, op1=ALU.mult)
            c.dma("sp", xo_d[b], ot[:], [ob], xod, nowaw=True, semof=ob)
        c.emit()
    return nc


def _gb(i, j):
    return 8 * j + i


def _shard_x(x, i):
    return np.ascontiguousarray(np.stack([x[128 * _gb(i, j):128 * _gb(i, j) + 128] for j in range(NB)]))


def _unshard_x(shards):
    out = np.zeros((NB * NCORE * 128, D), np.float32)
    for i in range(NCORE):
        for j in range(NB):
            out[128 * _gb(i, j):128 * _gb(i, j) + 128] = shards[i][j]
    return out


def _halo(x, i):
    xh = np.zeros((128, D), np.float32)
    tok = np.zeros((1, NCOL), np.float32)
    for j in range(NB):
        s = 128 * _gb(i, j)
        if s >= 16:
            xh[j * 16:(j + 1) * 16] = x[s - 16:s]
        tok[0, j * HC:(j + 1) * HC] = np.arange(s - 16, s + 128)
    return xh, tok


def _invf_row():
    a = 500000.0 ** (-np.arange(16, dtype=np.float32) * 2.0 / 32)
    b = 500000.0 ** (-np.arange(8, dtype=np.float32) * 2.0 / 16)
    return np.concatenate([a, b]).astype(np.float32)[None]


def _run(nc, maps):
    return run_bass_kernel_spmd(nc, maps, core_ids=list(range(NCORE))).results


def kernel(x, c, positions, norm_g, mod_w, mod_b, pool_w_in, pool_w_grp, pool_scale, pool_w_out,
           attn_w_in, attn_w_out, final_g):
    f32 = np.float32
    xc = np.ascontiguousarray(np.asarray(x, f32)[0])
    ccol = np.ascontiguousarray(np.asarray(c, f32)[0].reshape(16, 128).T)
    pos = np.asarray(positions)[0].astype(np.int32)
    ncs = {}

    def prog(name, fn):
        if name not in ncs:
            ncs[name] = fn()
        return ncs[name]

    for li in range(4):
        j2 = li // 2
        common = dict(ccol=ccol, modw=np.ascontiguousarray(mod_w[li], f32), modb=np.asarray(mod_b[li], f32)[None],
                      g=np.asarray(norm_g[li], f32)[None])
        if li % 2 == 0:
            maps = []
            lscol = np.ascontiguousarray(np.asarray(pool_scale[j2], f32).reshape(16, 128).T)
            for i in range(NCORE):
                xh, tok = _halo(xc, i)
                maps.append(dict(xs=_shard_x(xc, i), xh=xh, tokrow=tok, win=np.asarray(pool_w_in[j2], f32),
                                 wgrp=np.asarray(pool_w_grp[j2], f32), lscol=lscol,
                                 wout=np.asarray(pool_w_out[j2], f32), **common))
            res = _run(prog("pool", build_pool), maps)
            xc = _unshard_x([r["xo"] for r in res])
        else:
            maps = []
            for i in range(NCORE):
                posc = np.ascontiguousarray(
                    np.stack([pos[128 * _gb(i, j):128 * _gb(i, j) + 128] for j in range(NB)], axis=1))
                maps.append(dict(xs=_shard_x(xc, i), win=np.asarray(attn_w_in[j2], f32), posc=posc,
                                 invf=_invf_row(), **common))
            r1 = _run(prog("a1", build_a1), maps)
            S = NB * NCORE * 128
            bf = ml_dtypes.bfloat16
            KTg = np.zeros((NH, 128, S), bf)
            Vg = np.zeros((S, D), bf)
            KITg = np.zeros((64, S), bf)
            for i in range(NCORE):
                for j in range(NB):
                    gs = slice(128 * _gb(i, j), 128 * _gb(i, j) + 128)
                    ls = slice(128 * j, 128 * j + 128)
                    KTg[:, :, gs] = r1[i]["KT"][:, :, ls]
                    Vg[gs] = r1[i]["V"][ls]
                    KITg[:, gs] = r1[i]["KIT"][:, ls]
            Vg2 = np.ascontiguousarray(Vg.reshape(64, 128, NH, 128).transpose(2, 1, 0, 3))
            maps = []
            for i in range(NCORE):
                maps.append(dict(xs=_shard_x(xc, i), GBC=r1[i]["GBC"], QT=r1[i]["QT"], SG=r1[i]["SG"], QIT=r1[i]["QIT"],
                                 WI=r1[i]["WI"], KTg=KTg, Vg=Vg2, KITg=KITg,
                                 trel=(128 * i + np.arange(128, dtype=f32))[:, None],
                                 wout=np.asarray(attn_w_out[j2], f32)))
            r2 = _run(prog("a2", build_a2), maps)
            xc = _unshard_x([r["xo"] for r in r2])
    maps = [dict(xs=_shard_x(xc, i), g=np.asarray(final_g, f32)[None]) for i in range(NCORE)]
    rf = _run(prog("final", build_final), maps)
    out = _unshard_x([r["xo"] for r in rf])
    return out[None].astype(np.float32)
```
